# Optimizing a Trainium2 kernel written in Bass

```python
import math
import jax, jax.numpy as jnp
from jax import lax
import numpy as np

D_MODEL = 4096
BATCH = 2
SEQ = 8192
DEPTH = 4

HGRN_HEADS = 8
HGRN_KDIM = 128
HGRN_VDIM = 128
HGRN_WIDTH = HGRN_HEADS * HGRN_KDIM
HGRN_CHUNK = 64
DIFF_HEADS = 8
DIFF_HEAD_DIM = 64
DIFF_WIDTH = DIFF_HEADS * 2 * DIFF_HEAD_DIM
Q_BLOCK = 128
N_BUCKETS = 32
MAX_DISTANCE = 128
RET_HEADS = 8
RET_KDIM = 128
RET_VDIM = 256
RET_QK_WIDTH = RET_HEADS * RET_KDIM
RET_V_WIDTH = RET_HEADS * RET_VDIM
RET_CHUNK = 128
ROPE_BASE = 10000.0
GATE_RANK = 256
N_BRANCH = 3
FFN_HIDDEN = ((8 * D_MODEL // 3 + 255) // 256) * 256
RMS_EPS = 1e-6
IN_WIDTHS = (HGRN_WIDTH, HGRN_WIDTH, HGRN_WIDTH, HGRN_WIDTH,
             DIFF_WIDTH, DIFF_WIDTH, DIFF_WIDTH,
             RET_QK_WIDTH, RET_QK_WIDTH, RET_V_WIDTH, RET_V_WIDTH,
             GATE_RANK)
IN_TOTAL = sum(IN_WIDTHS)

kernel_name = "hybrid_hgrn2_diffattn_retention_block"

F32 = jnp.float32


def rms_norm(x, w):
    xf = x.astype(F32)
    y = xf * lax.rsqrt(jnp.mean(xf * xf, axis=-1, keepdims=True) + RMS_EPS)
    return (y * w.astype(F32)).astype(x.dtype)


def t5_bucket(n):
    n = jnp.maximum(n, 0)
    max_exact = N_BUCKETS // 2
    nf = jnp.maximum(n, 1).astype(F32)
    large = max_exact + (jnp.log(nf / max_exact) / math.log(MAX_DISTANCE / max_exact)
                         * (N_BUCKETS - max_exact)).astype(jnp.int32)
    large = jnp.minimum(large, N_BUCKETS - 1)
    return jnp.where(n < max_exact, n, large)


def hgrn2_mixer(q, f, i, g, lb, norm_w):
    B, S, _ = q.shape
    H, C, dk, dv = HGRN_HEADS, HGRN_CHUNK, HGRN_KDIM, HGRN_VDIM
    N = S // C
    lbf = lb.astype(F32)
    qf = jax.nn.silu(q.astype(F32)) * dk ** -0.5
    forget = lbf + (1.0 - lbf) * jax.nn.sigmoid(f.astype(F32))
    kf = 1.0 - forget
    logf = jnp.log(forget)

    def to_chunks(t, d):
        return t.reshape(B, N, C, H, d).transpose(1, 0, 3, 2, 4)

    qc, kc, lfc = to_chunks(qf, dk), to_chunks(kf, dk), to_chunks(logf, dk)
    ic = to_chunks(i.astype(F32), dv)
    causal = jnp.tril(jnp.ones((C, C), bool))[None, None, :, :, None]

    def step(state, inp):
        qb, kb, ib, lf = inp
        b = jnp.cumsum(lf, axis=2)
        pair = b[:, :, :, None, :] - b[:, :, None, :, :]
        decay = jnp.exp(jnp.where(causal, pair, -jnp.inf))
        attn = jnp.einsum('bhtd,bhsd,bhtsd->bhts', qb, kb, decay)
        o = jnp.einsum('bhts,bhse->bhte', attn, ib) + \
            jnp.einsum('bhtd,bhde->bhte', qb * jnp.exp(b), state)
        b_last = b[:, :, -1:, :]
        new_state = jnp.exp(b_last[:, :, 0, :, None]) * state + \
            jnp.einsum('bhsd,bhse->bhde', kb * jnp.exp(b_last - b), ib)
        return new_state, o

    state0 = jnp.zeros((B, H, dk, dv), F32)
    _, o = lax.scan(step, state0, (qc, kc, ic, lfc))
    o = o.transpose(1, 0, 3, 2, 4).reshape(B, S, H, dv)
    o = rms_norm(o, norm_w).reshape(B, S, H * dv) * jax.nn.silu(g.astype(F32))
    return o.astype(q.dtype)


def diff_attention(q, k, v, rel_bias, lam_params, lam_init, norm_w):
    B, S, _ = q.shape
    H, dh = DIFF_HEADS, DIFF_HEAD_DIM
    qf = q.reshape(B, S, H, 2, dh).astype(F32) * dh ** -0.5
    kf = k.reshape(B, S, H, 2, dh).astype(F32)
    vf = v.reshape(B, S, H, 2 * dh).astype(F32)
    lp = lam_params.astype(F32)
    lam = jnp.exp(jnp.sum(lp[0] * lp[1])) - jnp.exp(jnp.sum(lp[2] * lp[3])) + lam_init
    key_pos = jnp.arange(S)
    table = rel_bias.astype(F32)

    def block(qi):
        qb = lax.dynamic_slice_in_dim(qf, qi * Q_BLOCK, Q_BLOCK, axis=1)
        rel = (qi * Q_BLOCK + jnp.arange(Q_BLOCK))[:, None] - key_pos[None, :]
        bias = jnp.transpose(table[t5_bucket(rel)], (2, 0, 1))
        logits = jnp.einsum('bqhmd,bkhmd->bmhqk', qb, kf) + bias[None, None]
        logits = jnp.where(rel >= 0, logits, -jnp.inf)
        p = jax.nn.softmax(logits, axis=-1)
        w = p[:, 0] - lam * p[:, 1]
        return jnp.einsum('bhqk,bkhe->bqhe', w, vf)

    out = lax.map(block, jnp.arange(S // Q_BLOCK))
    out = out.transpose(1, 0, 2, 3, 4).reshape(B, S, H, 2 * dh)
    out = rms_norm(out, norm_w) * (1.0 - lam_init)
    return out.reshape(B, S, H * 2 * dh).astype(q.dtype)


def rotary(x):
    S, d = x.shape[1], x.shape[-1]
    half = d // 2
    inv = 1.0 / (ROPE_BASE ** (jnp.arange(half, dtype=F32) / half))
    ang = jnp.arange(S, dtype=F32)[:, None] * inv[None, :]
    cos, sin = jnp.cos(ang)[None, :, None, :], jnp.sin(ang)[None, :, None, :]
    x1, x2 = x[..., :half], x[..., half:]
    return jnp.concatenate([x1 * cos - x2 * sin, x1 * sin + x2 * cos], axis=-1)


def retention(q, k, v, g, norm_w):
    B, S, _ = q.shape
    H, C, dk, dv = RET_HEADS, RET_CHUNK, RET_KDIM, RET_VDIM
    N = S // C
    qf = rotary(q.reshape(B, S, H, dk).astype(F32))
    kf = rotary(k.reshape(B, S, H, dk).astype(F32)) * dk ** -0.5
    vf = v.reshape(B, S, H, dv).astype(F32)
    log_gamma = jnp.log(1.0 - 2.0 ** (-5.0 - jnp.arange(H, dtype=F32)))
    qc, kc = qf.reshape(B, N, C, H, dk), kf.reshape(B, N, C, H, dk)
    vc = vf.reshape(B, N, C, H, dv)
    idx = jnp.arange(C, dtype=F32)
    diff = idx[:, None] - idx[None, :]
    tril = diff >= 0
    D = jnp.exp(jnp.where(tril[None], diff[None] * log_gamma[:, None, None], -jnp.inf))
    scores = jnp.einsum('bnthd,bnshd->bnhts', qc, kc) * D[None, None]
    o_intra = jnp.einsum('bnhts,bnshe->bnthe', scores, vc)
    zeta = jnp.exp((C - 1 - idx)[:, None] * log_gamma[None, :])
    U = jnp.einsum('bnshd,sh,bnshe->nbhde', kc, zeta, vc)
    gamma_c = jnp.exp(C * log_gamma)[None, :, None, None]

    def step(R, U_n):
        return gamma_c * R + U_n, R

    _, R_prev = lax.scan(step, jnp.zeros((B, H, dk, dv), F32), U)
    xi = jnp.exp((idx + 1)[:, None] * log_gamma[None, :])
    o_inter = jnp.einsum('bnthd,nbhde,th->bnthe', qc, R_prev, xi)
    o = (o_intra + o_inter).reshape(B, S, H, dv)
    o = rms_norm(o, norm_w).reshape(B, S, H * dv) * jax.nn.silu(g.astype(F32))
    return o.astype(q.dtype)


def setup_inputs(seed: int = 0) -> dict:
    key = jax.random.key(seed)
    ks = jax.random.split(key, 24)

    def nrm(k, shape, fan_in):
        return jax.random.normal(k, shape, F32) * fan_in ** -0.5

    def gain(k, shape):
        return 1.0 + 0.02 * jax.random.normal(k, shape, F32)

    return {
        "x": jax.random.normal(ks[0], (BATCH, SEQ, D_MODEL), F32),
        "attn_norm_w": gain(ks[1], (DEPTH, D_MODEL)),
        "w_in": nrm(ks[2], (DEPTH, D_MODEL, IN_TOTAL), D_MODEL),
        "lb_logits": 0.5 * jax.random.normal(ks[3], (DEPTH, HGRN_WIDTH), F32),
        "hgrn_norm_w": gain(ks[4], (DEPTH, HGRN_VDIM)),
        "rel_bias": 0.5 * jax.random.normal(ks[5], (N_BUCKETS, DIFF_HEADS), F32),
        "diff_lambda": 0.1 * jax.random.normal(ks[6], (DEPTH, 4, DIFF_HEAD_DIM), F32),
        "diff_norm_w": gain(ks[7], (DEPTH, 2 * DIFF_HEAD_DIM)),
        "ret_norm_w": gain(ks[8], (DEPTH, RET_VDIM)),
        "w_gate_up": nrm(ks[9], (DEPTH, GATE_RANK, N_BRANCH * D_MODEL), GATE_RANK),
        "b_gate": 0.01 * jax.random.normal(ks[10], (DEPTH, N_BRANCH * D_MODEL), F32),
        "w_br_hgrn": nrm(ks[11], (DEPTH, HGRN_WIDTH, D_MODEL), HGRN_WIDTH),
        "w_br_diff": nrm(ks[12], (DEPTH, DIFF_WIDTH, D_MODEL), DIFF_WIDTH),
        "w_br_ret": nrm(ks[13], (DEPTH, RET_V_WIDTH, D_MODEL), RET_V_WIDTH),
        "w_o": nrm(ks[14], (DEPTH, D_MODEL, D_MODEL), D_MODEL),
        "ffn_norm_w": gain(ks[15], (DEPTH, D_MODEL)),
        "w_ffn_gate": nrm(ks[16], (DEPTH, D_MODEL, FFN_HIDDEN), D_MODEL),
        "w_ffn_up": nrm(ks[17], (DEPTH, D_MODEL, FFN_HIDDEN), D_MODEL),
        "w_ffn_down": nrm(ks[18], (DEPTH, FFN_HIDDEN, D_MODEL), FFN_HIDDEN),
        "final_norm_w": gain(ks[19], (D_MODEL,)),
    }


def reference(x, attn_norm_w, w_in, lb_logits, hgrn_norm_w, rel_bias, diff_lambda, diff_norm_w,
              ret_norm_w, w_gate_up, b_gate, w_br_hgrn, w_br_diff, w_br_ret, w_o, ffn_norm_w,
              w_ffn_gate, w_ffn_up, w_ffn_down, final_norm_w):
    dt = x.dtype
    split_points = [int(p) for p in np.cumsum(IN_WIDTHS)[:-1]]
    lb_sm = jax.nn.softmax(lb_logits.astype(F32), axis=0)
    lower_bounds = jnp.cumsum(lb_sm, axis=0) - lb_sm[0:1]
    for l in range(DEPTH):
        h = rms_norm(x, attn_norm_w[l])
        proj = h @ w_in[l]
        hq, hf, hi, hg, dq, dk, dv, rq, rk, rv, rg, gd = jnp.split(proj, split_points, axis=-1)
        y_h = hgrn2_mixer(hq, hf, hi, hg, lower_bounds[l], hgrn_norm_w[l])
        lam_init = 0.8 - 0.6 * math.exp(-0.3 * l)
        y_d = diff_attention(dq, dk, dv, rel_bias, diff_lambda[l], lam_init, diff_norm_w[l])
        y_r = retention(rq, rk, rv, rg, ret_norm_w[l])
        gates = jax.nn.sigmoid((gd @ w_gate_up[l] + b_gate[l]).astype(F32)).astype(dt)
        g_h, g_d, g_r = jnp.split(gates, N_BRANCH, axis=-1)
        merged = g_h * (y_h @ w_br_hgrn[l]) + g_d * (y_d @ w_br_diff[l]) + g_r * (y_r @ w_br_ret[l])
        x = x + merged @ w_o[l]
        h = rms_norm(x, ffn_norm_w[l])
        x = x + (jax.nn.silu(h @ w_ffn_gate[l]) * (h @ w_ffn_up[l])) @ w_ffn_down[l]
    return rms_norm(x, final_norm_w)
```

```python
import contextlib, numpy as np
import concourse.bass as bass
import concourse.mybir as mybir
from concourse.bass_utils import run_bass_kernel_spmd
F32 = mybir.dt.float32; BF16 = mybir.dt.bfloat16
ALU = mybir.AluOpType; AF = mybir.ActivationFunctionType; AX = mybir.AxisListType

class Sched:
    ENGS = ("pe", "act", "dve", "pool", "sp")
    def __init__(self, nc, es, n_dma_sems=40):
        self.nc = nc; self.es = es
        self.streams = {e: [] for e in self.ENGS}
        self.esem = {e: es.enter_context(nc.semaphore("es_" + e)) for e in ("pe", "act", "dve", "pool")}
        self.ecnt = {e: 0 for e in self.esem}
        self.waited = {e: {} for e in self.ENGS}
        self.lastw = {}; self.reads = {}
        self.dsem = {}; self.dcnt = {}
        self.n_dma = 0
    def _dma_sem(self, key):
        if key not in self.dsem:
            self.dsem[key] = self.es.enter_context(self.nc.semaphore("ds_%d" % len(self.dsem)))
            self.dcnt[key] = 0
        return self.dsem[key]
    def _deps(self, eng, reads, writes):
        deps = []
        for b in list(reads) + list(writes):
            if b in self.lastw: deps.append(self.lastw[b])
        for b in writes:
            deps.extend(self.reads.get(b, ()))
        need = {}
        for (sem, val) in deps:
            k = id(sem)
            if self.waited[eng].get(k, 0) >= val: continue
            if k not in need or need[k][1] < val: need[k] = (sem, val)
        for k, (sem, val) in need.items():
            self.waited[eng][k] = val
            self.streams[eng].append(("wait", sem, val))
    def _commit(self, tick, reads, writes):
        for b in writes:
            self.lastw[b] = tick; self.reads[b] = []
        for b in reads:
            self.reads.setdefault(b, []).append(tick)
    def op(self, eng, fn, reads=(), writes=()):
        self._deps(eng, reads, writes)
        self.ecnt[eng] += 1
        tick = (self.esem[eng], self.ecnt[eng])
        self.streams[eng].append(("op", fn, self.esem[eng], 1))
        self.waited[eng][id(self.esem[eng])] = max(self.waited[eng].get(id(self.esem[eng]), 0), 0)
        self._commit(tick, reads, writes)
    def group(self, eng, fns, reads=(), writes=()):
        self._deps(eng, reads, writes)
        for f in fns[:-1]:
            self.streams[eng].append(("op", f, None, 0))
        self.ecnt[eng] += 1
        tick = (self.esem[eng], self.ecnt[eng])
        self.streams[eng].append(("op", fns[-1], self.esem[eng], 1))
        self._commit(tick, reads, writes)
    def dma(self, eng, fn, semkey, reads=(), writes=()):
        sem = self._dma_sem(semkey)
        self._deps(eng, reads, writes)
        self.dcnt[semkey] += 16
        tick = (sem, self.dcnt[semkey])
        self.streams[eng].append(("op", fn, sem, 16))
        self._commit(tick, reads, writes)
    def finish(self):
        for key, sem in self.dsem.items():
            if self.dcnt[key]: self.streams["sp"].append(("wait", sem, self.dcnt[key]))
        for e, sem in self.esem.items():
            if self.ecnt[e]: self.streams["sp"].append(("wait", sem, self.ecnt[e]))
    def replay(self):
        nc = self.nc
        def run(engobj, items):
            for it in items:
                if it[0] == "wait":
                    engobj.wait_ge(it[1], it[2])
                else:
                    ins = it[1](engobj)
                    if it[2] is not None:
                        ins.then_inc(it[2], it[3])
        with nc.Block() as block:
            @block.sync
            def _(e): run(e, self.streams["sp"])
            @block.tensor
            def _(e): run(e, self.streams["pe"])
            @block.scalar
            def _(e): run(e, self.streams["act"])
            @block.vector
            def _(e): run(e, self.streams["dve"])
            @block.gpsimd
            def _(e): run(e, self.streams["pool"])
    def stats(self):
        return {e: len(v) for e, v in self.streams.items()}

def sched_barrier(s):
    for eng in s.ENGS:
        for e2, sem in s.esem.items():
            v = s.ecnt[e2]
            if v and s.waited[eng].get(id(sem), 0) < v:
                s.streams[eng].append(("wait", sem, v)); s.waited[eng][id(sem)] = v
        for key, sem in s.dsem.items():
            v = s.dcnt[key]
            if v and s.waited[eng].get(id(sem), 0) < v:
                s.streams[eng].append(("wait", sem, v)); s.waited[eng][id(sem)] = v
    s.lastw = {}; s.reads = {}

def sched_flush(s):
    s.replay()
    for e in s.streams: s.streams[e] = []


D = 4096; KC = 32; TOK = 2048; TS = 512; NIN = 13568; FFN = 11008; GR = 256
EPS = 1e-6
WMAX = 16384

def f_mm(out, lhsT, rhs, start, stop):
    return lambda e: e.matmul(out, lhsT=lhsT, rhs=rhs, start=start, stop=stop)
def f_dma(out, in_):
    return lambda e: e.dma_start(out=out, in_=in_)
def f_act(out, in_, func, bias=None, scale=None):
    kw = {}
    if bias is not None: kw["bias"] = bias
    if scale is not None: kw["scale"] = scale
    return lambda e: e.activation(out=out, in_=in_, func=func, **kw)
def f_tt(out, in0, in1, op):
    return lambda e: e.tensor_tensor(out=out, in0=in0, in1=in1, op=op)
def f_ts(out, in0, s1, s2, op0, op1=None):
    if op1 is None:
        return lambda e: e.tensor_scalar(out=out, in0=in0, scalar1=s1, scalar2=None, op0=op0)
    return lambda e: e.tensor_scalar(out=out, in0=in0, scalar1=s1, scalar2=s2, op0=op0, op1=op1)
def f_stt(out, in0, scalar, in1, op0, op1):
    return lambda e: e.scalar_tensor_tensor(out=out, in0=in0, scalar=scalar, in1=in1, op0=op0, op1=op1)
def f_copy(out, in_):
    return lambda e: e.tensor_copy(out=out, in_=in_)
def f_recip(out, in_):
    return lambda e: e.reciprocal(out=out, in_=in_)
def f_memset(ap, v):
    return lambda e: e.memset(ap, v)

class Ctx:
    def __init__(self, nc, es, s, nps=7, gemm=True):
        self.nc = nc; self.es = es; self.s = s
        self.nps = nps; self.rr = 0
        if gemm:
            self.ps = [es.enter_context(nc.psum_tensor("ps%d" % i, [128, 512], F32)) for i in range(8)]
            self.wb = [es.enter_context(nc.sbuf_tensor("wb%d" % i, [128, WMAX], BF16)) for i in range(2)]
        self.wcount = 0
        self.ones = es.enter_context(nc.sbuf_tensor("ones", [128, 128], BF16))
        s.op("dve", f_memset(self.ones[:], 1.0), writes=["ones"])
        self._n = 0
    def sb(self, name, shape, dt):
        return self.es.enter_context(self.nc.sbuf_tensor(name, shape, dt))
    def next_ps(self):
        i = self.rr % self.nps; self.rr += 1
        return self.ps[i], ("ps", i)

def gemm(cx, groups, ncols, CB, epilogue, fo_base=0):
    s = cx.s
    ncb = (ncols + CB - 1) // CB
    secoff = []; off = 0
    for g in groups:
        secoff.append(off); off += g["nk"] * CB
    assert off <= WMAX, off
    def issue_w(cb):
        c0 = cb * CB; cw = min(CB, ncols - c0)
        slot = cx.wcount % 2; cx.wcount += 1
        for gi, g in enumerate(groups):
            sec = cx.wb[slot][:, secoff[gi]:secoff[gi] + g["nk"] * CB].rearrange("p (k c) -> p k c", c=CB)
            src = g["wap"](c0, cw).rearrange("(k p) n -> p k n", p=128)
            s.dma("pool", f_dma(sec[:, :, 0:cw], src), "wb%d_%d" % (slot, gi), writes=[("wb", slot, gi)])
        return slot
    slots = {}
    slots[0] = issue_w(0)
    if ncb > 1: slots[1] = issue_w(1)
    for cb in range(ncb):
        c0 = cb * CB; cw = min(CB, ncols - c0)
        slot = slots[cb]
        for fi in range(cw // 128):
            fo = fo_base + (c0 // 128) + fi
            outs = []
            for gi, g in enumerate(groups):
                sec = cx.wb[slot][:, secoff[gi]:secoff[gi] + g["nk"] * CB].rearrange("p (k c) -> p k c", c=CB)
                pt, pk = cx.next_ps()
                fns = [f_mm(pt[:], sec[:, kc, fi * 128:(fi + 1) * 128], g["rhs"](kc), kc == 0, kc == g["nk"] - 1)
                       for kc in range(g["nk"])]
                s.group("pe", fns, reads=[("wb", slot, gi)] + list(g["rkeys"]), writes=[pk])
                outs.append((pt, pk))
            epilogue(fo, outs)
        if cb + 2 < ncb:
            slots[cb + 2] = issue_w(cb + 2)

def emit_norm_prologue(cx, xsrc, t0, nwsb, hT, rstd, tag, x_from_sbuf=None):
    s = cx.s
    xTv = xsrc.rearrange("(kc p) t -> p kc t", p=128)
    pss = cx.ps[7]
    for kc in range(KC):
        xb = cx.xs[kc % 3]; sqb = cx.sq[kc % 2]
        s.dma("sp", f_dma(xb[:], xTv[:, kc, t0:t0 + TS]), "xs%d" % (kc % 3), writes=[("xs", kc % 3)])
        s.op("act", f_act(sqb[:], xb[:], AF.Square), reads=[("xs", kc % 3)], writes=[("sq", kc % 2)])
        s.op("dve", f_ts(hT[:, kc, :], xb[:], nwsb[:, kc:kc + 1], None, ALU.mult),
             reads=[("xs", kc % 3), "nw" + tag], writes=[("hT", kc)])
        s.group("pe", [f_mm(pss[:], cx.ones[:], sqb[:], kc == 0, kc == KC - 1)], reads=[("sq", kc % 2), "ones"], writes=[("ps", 7)])
    s.op("act", f_act(rstd[:], pss[:], AF.Sqrt, bias=EPS, scale=1.0 / D), reads=[("ps", 7)], writes=["rstd"])
    s.op("dve", f_recip(rstd[:], rstd[:]), reads=["rstd"], writes=["rstd"])

def emit_phase_a(cx, xT, w, anw, out, ncols=NIN):
    s = cx.s
    hT = cx.sb("a_hT", [128, KC, TS], BF16)
    cx.xs = [cx.sb("xs%d" % i, [128, TS], F32) for i in range(3)]
    cx.sq = [cx.sb("sq%d" % i, [128, TS], BF16) for i in range(2)]
    ob = [cx.sb("a_ob%d" % i, [128, TS], F32) for i in range(3)]
    rstd = cx.sb("a_rstd", [128, TS], F32)
    anw_sb = cx.sb("a_anw", [128, KC], F32)
    s.dma("sp", f_dma(anw_sb[:], anw), "anw", writes=["nwA"])
    cnt = [0]
    for st in range(TOK // TS):
        t0 = st * TS
        emit_norm_prologue(cx, xT, t0, anw_sb, hT, rstd, "A")
        def epi(fo, outs, t0=t0):
            pt, pk = outs[0]
            i = cnt[0] % 3; cnt[0] += 1
            s.op("dve" if fo % 2 == 0 else "act",
                 f_tt(ob[i][:], pt[:], rstd[:], ALU.mult) if fo % 2 == 0 else f_tt(ob[i][:], pt[:], rstd[:], ALU.mult),
                 reads=[pk, "rstd"], writes=[("ob", i)]) if False else \
            s.op("dve", f_tt(ob[i][:], pt[:], rstd[:], ALU.mult), reads=[pk, "rstd"], writes=[("ob", i)])
            s.dma("sp", f_dma(out[fo * 128:(fo + 1) * 128, t0:t0 + TS], ob[i][:]), "ob%d" % i, reads=[("ob", i)])
        grp = dict(nk=KC, rhs=lambda kc: hT[:, kc, :], rkeys=[("hT", kc) for kc in range(KC)],
                   wap=lambda c0, cw: w[:, c0:c0 + cw])
        gemm(cx, [grp], ncols, 512, epi)

def build_phase_a(ncols=NIN):
    nc = bass.Bass("TRN2", target_bir_lowering=False)
    xT = nc.dram_tensor("xT", [D, TOK], F32, kind="ExternalInput").ap()
    w = nc.dram_tensor("w", [D, ncols], F32, kind="ExternalInput").ap()
    anw = nc.dram_tensor("anw", [128, KC], F32, kind="ExternalInput").ap()
    out = nc.dram_tensor("projT", [ncols, TOK], F32, kind="ExternalOutput").ap()
    with contextlib.ExitStack() as es:
        s = Sched(nc, es)
        cx = Ctx(nc, es, s)
        emit_phase_a(cx, xT, w, anw, out, ncols)
        s.finish(); s.replay()
    return nc


HH = FFN // 2
NKH = HH // 128

def emit_phase_c(cx, yT, gdT, xT, wgu, bg, wbh, wbd, wbr, wo, fnw, wg, wu, wd, lnw, outT, last):
    s = cx.s
    bufA = cx.sb("bufA", [128, 32, TS], BF16)
    bufB = cx.sb("bufB", [128, NKH, TS], BF16)
    gdb = cx.sb("gdb", [128, 2, TS], BF16)
    cx.xs = [cx.sb("xs%d" % i, [128, TS], F32) for i in range(3)]
    cx.sq = [cx.sb("sq%d" % i, [128, TS], BF16) for i in range(2)]
    gt = [[cx.sb("gt%d_%d" % (i, j), [128, TS], F32) for j in range(3)] for i in range(2)]
    mt = [cx.sb("mt%d" % i, [128, TS], F32) for i in range(2)]
    tt = [cx.sb("tt%d" % i, [128, TS], F32) for i in range(2)]
    x1t = [cx.sb("x1t%d" % i, [128, TS], F32) for i in range(3)]
    tg = [cx.sb("tg%d" % i, [128, TS], F32) for i in range(2)]
    tsg = [cx.sb("tsg%d" % i, [128, TS], F32) for i in range(2)]
    tu = [cx.sb("tu%d" % i, [128, TS], F32) for i in range(2)]
    rstd = cx.sb("c_rstd", [128, TS], F32)
    bg_sb = cx.sb("bg_sb", [128, 96], F32)
    fnw_sb = cx.sb("fnw_sb", [128, KC], F32)
    lnw_sb = cx.sb("lnw_sb", [128, KC], F32)
    s.dma("sp", f_dma(bg_sb[:], bg), "bg", writes=["bg"])
    s.dma("sp", f_dma(fnw_sb[:], fnw), "fnw", writes=["fnw"])
    if last:
        s.dma("sp", f_dma(lnw_sb[:], lnw), "lnw", writes=["lnw"])
    yTv = yT.rearrange("(kc p) t -> p kc t", p=128)
    gdv = gdT.rearrange("(kc p) t -> p kc t", p=128)
    pss = cx.ps[7]
    cnt = {"x": 0, "p3": 0}
    for st in range(TOK // TS):
        t0 = st * TS
        for q in range(4):
            s.dma("pool", f_dma(bufA[:, q * 8:(q + 1) * 8, :], yTv[:, q * 8:(q + 1) * 8, t0:t0 + TS]), "yin%d" % q,
                  writes=[("bufA", kc) for kc in range(q * 8, (q + 1) * 8)])
        s.dma("pool", f_dma(gdb[:], gdv[:, :, t0:t0 + TS]), "gdin", writes=["gdb"])
        def epi1(fo, outs):
            i = fo % 2
            for br in range(3):
                pt, pk = outs[br]
                s.op("act", f_act(gt[i][br][:], pt[:], AF.Sigmoid, bias=bg_sb[:, br * 32 + fo:br * 32 + fo + 1]),
                     reads=[pk, "bg"], writes=[("gt", i, br)])
            s.op("dve", f_tt(mt[i][:], outs[3][0][:], gt[i][0][:], ALU.mult), reads=[outs[3][1], ("gt", i, 0)], writes=[("mt", i)])
            s.op("dve", f_tt(tt[i][:], outs[4][0][:], gt[i][1][:], ALU.mult), reads=[outs[4][1], ("gt", i, 1)], writes=[("tt", i)])
            s.op("pool", f_tt(mt[i][:], mt[i][:], tt[i][:], ALU.add), reads=[("mt", i), ("tt", i)], writes=[("mt", i)])
            s.op("dve", f_tt(tt[i][:], outs[5][0][:], gt[i][2][:], ALU.mult), reads=[outs[5][1], ("gt", i, 2), ("tt", i)], writes=[("tt", i)])
            s.op("pool", f_tt(bufB[:, fo, :], mt[i][:], tt[i][:], ALU.add), reads=[("mt", i), ("tt", i)], writes=[("bufB", fo)])
        groups = []
        for br in range(3):
            groups.append(dict(nk=2, rhs=lambda kc: gdb[:, kc, :], rkeys=["gdb"],
                               wap=(lambda c0, cw, br=br: wgu[:, br * D + c0: br * D + c0 + cw])))
        groups.append(dict(nk=8, rhs=lambda kc: bufA[:, kc, :], rkeys=[("bufA", k) for k in range(0, 8)], wap=lambda c0, cw: wbh[:, c0:c0 + cw]))
        groups.append(dict(nk=8, rhs=lambda kc: bufA[:, 8 + kc, :], rkeys=[("bufA", k) for k in range(8, 16)], wap=lambda c0, cw: wbd[:, c0:c0 + cw]))
        groups.append(dict(nk=16, rhs=lambda kc: bufA[:, 16 + kc, :], rkeys=[("bufA", k) for k in range(16, 32)], wap=lambda c0, cw: wbr[:, c0:c0 + cw]))
        gemm(cx, groups, D, 256, epi1)
        def epi2(fo, outs, t0=t0):
            pt, pk = outs[0]
            i = cnt["x"] % 3; cnt["x"] += 1
            s.dma("sp", f_dma(cx.xs[i][:], xT[fo * 128:(fo + 1) * 128, t0:t0 + TS]), "xs%d" % i, writes=[("xs", i)])
            s.op("dve", f_tt(x1t[i][:], pt[:], cx.xs[i][:], ALU.add), reads=[pk, ("xs", i)], writes=[("x1t", i)])
            s.dma("sp", f_dma(outT[fo * 128:(fo + 1) * 128, t0:t0 + TS], x1t[i][:]), "x1o%d" % i, reads=[("x1t", i)], writes=[("xacc", fo)])
            j = fo % 2
            s.op("act", f_act(cx.sq[j][:], x1t[i][:], AF.Square), reads=[("x1t", i)], writes=[("sq", j)])
            s.group("pe", [f_mm(pss[:], cx.ones[:], cx.sq[j][:], fo == 0, fo == KC - 1)], reads=[("sq", j), "ones"], writes=[("ps", 7)])
            s.op("pool", f_ts(bufA[:, fo, :], x1t[i][:], fnw_sb[:, fo:fo + 1], None, ALU.mult), reads=[("x1t", i), "fnw"], writes=[("bufA", fo)])
        grp = dict(nk=32, rhs=lambda kc: bufB[:, kc, :], rkeys=[("bufB", k) for k in range(32)], wap=lambda c0, cw: wo[:, c0:c0 + cw])
        gemm(cx, [grp], D, 512, epi2)
        s.op("act", f_act(rstd[:], pss[:], AF.Sqrt, bias=EPS, scale=1.0 / D), reads=[("ps", 7)], writes=["rstd"])
        s.op("dve", f_recip(rstd[:], rstd[:]), reads=["rstd"], writes=["rstd"])
        for hh in range(2):
            def epi3(fl, outs):
                i = cnt["p3"] % 2; cnt["p3"] += 1
                s.op("dve", f_tt(tg[i][:], outs[0][0][:], rstd[:], ALU.mult), reads=[outs[0][1], "rstd"], writes=[("tg", i)])
                s.op("act", f_act(tsg[i][:], tg[i][:], AF.Silu), reads=[("tg", i)], writes=[("tsg", i)])
                s.op("dve", f_tt(tu[i][:], outs[1][0][:], rstd[:], ALU.mult), reads=[outs[1][1], "rstd"], writes=[("tu", i)])
                s.op("pool", f_tt(bufB[:, fl, :], tsg[i][:], tu[i][:], ALU.mult), reads=[("tsg", i), ("tu", i)], writes=[("bufB", fl)])
            h0 = hh * HH
            g1 = dict(nk=32, rhs=lambda kc: bufA[:, kc, :], rkeys=[("bufA", k) for k in range(32)], wap=lambda c0, cw, h0=h0: wg[:, h0 + c0:h0 + c0 + cw])
            g2 = dict(nk=32, rhs=lambda kc: bufA[:, kc, :], rkeys=[("bufA", k) for k in range(32)], wap=lambda c0, cw, h0=h0: wu[:, h0 + c0:h0 + c0 + cw])
            gemm(cx, [g1, g2], HH, 256, epi3)
            fin = last and hh == 1
            def epi4(fo, outs, t0=t0, fin=fin):
                pt, pk = outs[0]
                i = cnt["x"] % 3; cnt["x"] += 1
                s.dma("sp", f_dma(cx.xs[i][:], outT[fo * 128:(fo + 1) * 128, t0:t0 + TS]), "xs%d" % i, reads=[("xacc", fo)], writes=[("xs", i)])
                s.op("dve", f_tt(x1t[i][:], pt[:], cx.xs[i][:], ALU.add), reads=[pk, ("xs", i)], writes=[("x1t", i)])
                s.dma("sp", f_dma(outT[fo * 128:(fo + 1) * 128, t0:t0 + TS], x1t[i][:]), "x1o%d" % i, reads=[("x1t", i)], writes=[("xacc", fo)])
                if fin:
                    j = fo % 2
                    s.op("act", f_act(cx.sq[j][:], x1t[i][:], AF.Square), reads=[("x1t", i)], writes=[("sq", j)])
                    s.group("pe", [f_mm(pss[:], cx.ones[:], cx.sq[j][:], fo == 0, fo == KC - 1)], reads=[("sq", j), "ones"], writes=[("ps", 7)])
            g4 = dict(nk=NKH, rhs=lambda kc: bufB[:, kc, :], rkeys=[("bufB", k) for k in range(NKH)], wap=lambda c0, cw, h0=h0: wd[h0:h0 + HH, c0:c0 + cw])
            gemm(cx, [g4], D, 256, epi4)
        if last:
            s.op("act", f_act(rstd[:], pss[:], AF.Sqrt, bias=EPS, scale=1.0 / D), reads=[("ps", 7)], writes=["rstd"])
            s.op("dve", f_recip(rstd[:], rstd[:]), reads=["rstd"], writes=["rstd"])
            for fo in range(KC):
                i = cnt["x"] % 3; cnt["x"] += 1
                s.dma("sp", f_dma(cx.xs[i][:], outT[fo * 128:(fo + 1) * 128, t0:t0 + TS]), "xs%d" % i, reads=[("xacc", fo)], writes=[("xs", i)])
                s.op("dve", f_stt(x1t[i][:], cx.xs[i][:], lnw_sb[:, fo:fo + 1], rstd[:], ALU.mult, ALU.mult), reads=[("xs", i), "lnw", "rstd"], writes=[("x1t", i)])
                s.dma("sp", f_dma(outT[fo * 128:(fo + 1) * 128, t0:t0 + TS], x1t[i][:]), "x1o%d" % i, reads=[("x1t", i)], writes=[("xacc", fo)])

def build_phase_c(last):
    nc = bass.Bass("TRN2", target_bir_lowering=False)
    di = lambda name, shape: nc.dram_tensor(name, shape, F32, kind="ExternalInput").ap()
    yT = di("yT", [D, TOK]); gdT = di("gdT", [GR, TOK]); xT = di("xT", [D, TOK])
    wgu = di("wgu", [GR, 3 * D]); bg = di("bg", [128, 96])
    wbh = di("wbh", [1024, D]); wbd = di("wbd", [1024, D]); wbr = di("wbr", [2048, D]); wo = di("wo", [D, D])
    fnw = di("fnw", [128, KC]); wg = di("wg", [D, FFN]); wu = di("wu", [D, FFN]); wd = di("wd", [FFN, D])
    lnw = di("lnw", [128, KC])
    outT = nc.dram_tensor("outT", [D, TOK], F32, kind="ExternalOutput").ap()
    with contextlib.ExitStack() as es:
        s = Sched(nc, es)
        cx = Ctx(nc, es, s)
        emit_phase_c(cx, yT, gdT, xT, wgu, bg, wbh, wbd, wbr, wo, fnw, wg, wu, wd, lnw, outT, last)
        s.finish(); s.replay()
    return nc

def sigmoid(v): return 1 / (1 + np.exp(-v))
def ref_phase_c(y, gd, x, wgu, bgv, wbh, wbd, wbr, wo, fnwv, wg, wu, wd, lnwv, last):
    gates = sigmoid(gd @ wgu + bgv)
    gh, gdd, gr = gates[:, :D], gates[:, D:2 * D], gates[:, 2 * D:]
    merged = gh * (y[:, :1024] @ wbh) + gdd * (y[:, 1024:2048] @ wbd) + gr * (y[:, 2048:] @ wbr)
    x1 = x + merged @ wo
    h = x1 / np.sqrt((x1 * x1).mean(-1, keepdims=True) + EPS) * fnwv
    g = h @ wg
    x2 = x1 + ((g * sigmoid(g)) * (h @ wu)) @ wd
    if last:
        x2 = x2 / np.sqrt((x2 * x2).mean(-1, keepdims=True) + EPS) * lnwv
    return x2


S = 8192; SEG = 1024; NCH = SEG // 32
CL = 32

def f_scan(out, d0, d1, init, op0, op1):
    return lambda e: e.tensor_tensor_scan(out=out, data0=d0, data1=d1, initial=init, op0=op0, op1=op1)
def f_reduce(out, in_, axis, op):
    return lambda e: e.tensor_reduce(out=out, in_=in_, axis=axis, op=op)
def f_tr(out, in_, ident):
    return lambda e: e.transpose(out, in_, ident)
def f_acta(out, in_, func, accum):
    return lambda e: e.activation(out=out, in_=in_, func=func, accum_out=accum)

def bc_chunk(ap2d, n):
    return ap2d.unsqueeze(2).to_broadcast([ap2d.shape[0], ap2d.shape[1], n])
def bc_mid(ap2d, m):
    return ap2d.unsqueeze(1).to_broadcast([ap2d.shape[0], m, ap2d.shape[1]])

class LA:
    def __init__(self, cx, tag, nh, dv, ps_a, ps_o, ps_t, ps_u, mask, ident):
        self.cx = cx; self.tag = tag; self.nh = nh; self.dv = dv
        self.ps_a, self.ps_o, self.ps_t, self.ps_u = ps_a, ps_o, ps_t, ps_u
        self.mask = mask; self.ident = ident
        sb = cx.sb
        self.S = [sb(tag + "S%d" % j, [128, dv], F32) for j in range(nh)]
        self.Sb = [sb(tag + "Sb%d" % j, [128, dv], BF16) for j in range(nh)]
        self.A = [[sb(tag + "A%d_%d" % (j, i), [CL, CL], BF16) for i in range(2)] for j in range(nh)]
        self.Kt = [[sb(tag + "Kk%d_%d" % (j, i), [CL, 128], BF16) for i in range(2)] for j in range(nh)]
        s = cx.s
        for j in range(nh):
            s.op("dve", f_memset(self.S[j][:], 0.0), writes=[(tag, "S", j)])
            s.op("dve", f_memset(self.Sb[j][:], 0.0), writes=[(tag, "Sb", j)])
        self.n = 0
    def run_segment(self, Qt, Kt, Qh, Kh, dec, V, O, keys):
        s = self.cx.s; tag = self.tag; dv = self.dv
        for c in range(NCH):
            cs = slice(c * CL, (c + 1) * CL)
            for j in range(self.nh):
                i = self.n % 2
                k = keys[j]
                pa = self.ps_a[j][i]; po = self.ps_o[j][i]; ptt = self.ps_t[j][i]; pu = self.ps_u[j]
                A = self.A[j][i]; Kk = self.Kt[j][i]
                s.group("pe", [f_mm(pa[:], Kt[j][:, cs], Qt[j][:, cs], True, True)], reads=[k["Kt"], k["Qt"]], writes=[(tag, "pa", j, i)])
                s.op("dve", f_tt(A[:], pa[:], self.mask[:], ALU.mult), reads=[(tag, "pa", j, i), "mask"], writes=[(tag, "A", j, i)])
                s.group("pe", [f_tr(ptt[:], Kh[j][:, cs], self.ident[:])], reads=[k["Kh"], "ident"], writes=[(tag, "pt", j, i)])
                s.op("act", f_copy_act(Kk[:], ptt[:]), reads=[(tag, "pt", j, i)], writes=[(tag, "Kk", j, i)])
                s.group("pe", [f_mm(po[:], A[:], V[j][:, c, :], True, False),
                               f_mm(po[:], Qh[j][:, cs], self.Sb[j][:], False, True)],
                        reads=[(tag, "A", j, i), k["V"], k["Qh"], (tag, "Sb", j)], writes=[(tag, "po", j, i)])
                s.op("act", f_copy_act(O[j][:, c, :], po[:]), reads=[(tag, "po", j, i)], writes=[k["O"]])
                s.group("pe", [f_mm(pu[:], Kk[:], V[j][:, c, :], True, True)], reads=[(tag, "Kk", j, i), k["V"]], writes=[(tag, "pu", j)])
                s.op("dve", f_stt(self.S[j][:], self.S[j][:], dec[j][:, c:c + 1], pu[:], ALU.mult, ALU.add),
                     reads=[(tag, "S", j), k["dec"], (tag, "pu", j)], writes=[(tag, "S", j)])
                s.op("act", f_copy_act(self.Sb[j][:], self.S[j][:]), reads=[(tag, "S", j)], writes=[(tag, "Sb", j)])
            self.n += 1

def la_psum(cx, dv):
    pb, pbt = cx.pb, cx.pbt
    ps_a = [[pb[0][0:CL, (2 * j + i) * CL:(2 * j + i + 1) * CL] for i in range(2)] for j in range(2)]
    ps_o = [[pb[1 + j][0:CL, i * 256:i * 256 + dv] for i in range(2)] for j in range(2)]
    ps_t = [[pbt[0:CL, (2 * j + i) * 128:(2 * j + i + 1) * 128] for i in range(2)] for j in range(2)]
    ps_u = [pb[4 + j][:, 0:dv] for j in range(2)]
    return ps_a, ps_o, ps_t, ps_u

def alloc_b_psum(cx):
    cx.pb = [cx.es.enter_context(cx.nc.psum_tensor("pb%d" % i, [128, 512], F32)) for i in range(7)]
    cx.pbt = cx.es.enter_context(cx.nc.psum_tensor("pbt", [128, 1024], BF16))

def f_copy_act(out, in_):
    return lambda e: e.copy(out=out, in_=in_)

def la_backend(cx, s, tag, j, O, g_src, nw_bc, ydst, dv, bufs, okey):
    gt, ssq, rs = bufs
    s.op("act", f_act(gt[:], O[:], AF.Square), reads=[okey], writes=[(tag, "gt")])
    s.op("dve", f_reduce(ssq[:], gt[:], AX.X, ALU.add), reads=[(tag, "gt")], writes=[(tag, "ssq")])
    s.op("act", f_act(rs[:], ssq[:], AF.Sqrt, bias=EPS, scale=1.0 / dv), reads=[(tag, "ssq")], writes=[(tag, "rs")])
    s.op("dve", f_recip(rs[:], rs[:]), reads=[(tag, "rs")], writes=[(tag, "rs")])
    s.dma("sp", f_dma(gt[:], g_src), tag + "g", reads=[(tag, "gt")], writes=[(tag, "gt")])
    s.op("act", f_act(gt[:], gt[:], AF.Silu), reads=[(tag, "gt")], writes=[(tag, "gt")])
    s.op("dve", f_tt(gt[:], gt[:], bc_mid(nw_bc, NCH), ALU.mult), reads=[(tag, "gt"), "nw" + tag], writes=[(tag, "gt")])
    s.op("dve", f_tt(O[:], O[:], bc_chunk(rs[:], dv), ALU.mult), reads=[okey, (tag, "rs")], writes=[okey])
    s.op("dve", f_tt(gt[:], gt[:], O[:], ALU.mult), reads=[(tag, "gt"), okey], writes=[(tag, "gt")])
    s.dma("sp", f_dma(ydst, gt[:]), tag + "y", reads=[(tag, "gt")])

def emit_hgrn(cx, layer, hq, hf, hi, hg, lbl, hnw, yh, consts):
    s = cx.s; sb = cx.sb; nc = cx.nc; es = cx.es
    mask, ident, rst = consts["mask"], consts["ident"], consts["rst"]
    ps_a, ps_o, ps_t, ps_u = la_psum(cx, 128)
    la = LA(cx, "H", 2, 128, ps_a, ps_o, ps_t, ps_u, mask, ident)
    lb = []; oml = []
    for j in range(2):
        lg = sb("h_lg%d" % j, [128, 4], F32); ssum = sb("h_ls%d" % j, [128, 1], F32)
        lbj = sb("h_lb%d" % j, [128, 1], F32); omlj = sb("h_oml%d" % j, [128, 1], F32)
        s.dma("sp", f_dma(lg[:], lbl[j]), "hlb%d" % j, writes=[("lg", j)])
        s.op("act", f_act(lg[:], lg[:], AF.Exp), reads=[("lg", j)], writes=[("lg", j)])
        s.op("dve", f_reduce(ssum[:], lg[:], AX.X, ALU.add), reads=[("lg", j)], writes=[("ls", j)])
        s.op("dve", f_recip(ssum[:], ssum[:]), reads=[("ls", j)], writes=[("ls", j)])
        if layer == 0:
            s.op("dve", f_memset(lbj[:], 0.0), writes=[("lb", j)])
        else:
            s.op("dve", f_reduce(lbj[:], lg[:, 1:layer + 1], AX.X, ALU.add), reads=[("lg", j)], writes=[("lb", j)])
            s.op("dve", f_tt(lbj[:], lbj[:], ssum[:], ALU.mult), reads=[("lb", j), ("ls", j)], writes=[("lb", j)])
        s.op("dve", f_ts(omlj[:], lbj[:], -1.0, 1.0, ALU.mult, ALU.add), reads=[("lb", j)], writes=[("oml", j)])
        lb.append(lbj); oml.append(omlj)
    nw_bc = sb("h_nw", [CL, 128], F32)
    s.dma("sp", f_dma(nw_bc[:], hnw.partition_broadcast(CL)), "hnw", writes=["nwH"])
    W = {}
    for nm in ("q", "f", "b", "t1", "t2"):
        W[nm, 0] = W[nm, 1] = sb("h_%s" % nm, [128, SEG], F32)
    for j in range(2):
        for nm in ("Qt", "Kt", "Qh", "Kh"):
            W[nm, j] = sb("h_%s%d" % (nm, j), [128, SEG], BF16)
        W["dec", j] = sb("h_dec%d" % j, [128, NCH], F32)
        W["V", j] = sb("h_V%d" % j, [CL, NCH, 128], BF16)
        W["O", j] = sb("h_O%d" % j, [CL, NCH, 128], F32)
    bufs = (sb("h_gt", [CL, NCH, 128], F32), sb("h_ssq", [CL, NCH], F32), sb("h_rs", [CL, NCH], F32))
    sc = 128 ** -0.5
    for seg in range(S // SEG):
        t0 = seg * SEG
        keys = []
        for j in range(2):
            q, f, b, t1, t2 = (W[n, j] for n in ("q", "f", "b", "t1", "t2"))
            K = lambda n, j=j: ("H", n, j) if n in ("Qt", "Kt", "Qh", "Kh", "dec", "V", "O") else ("H", n)
            s.dma("sp", f_dma(q[:], hq[j][:, t0:t0 + SEG]), "hq%d" % j, writes=[K("q")])
            s.dma("sp", f_dma(f[:], hf[j][:, t0:t0 + SEG]), "hf%d" % j, writes=[K("f")])
            s.dma("pool", f_dma(W["V", j][:], hi[j][t0:t0 + SEG, :].rearrange("(c p) e -> p c e", p=CL)), "hV%d" % j, writes=[K("V")])
            s.op("act", f_act(q[:], q[:], AF.Silu), reads=[K("q")], writes=[K("q")])
            s.op("act", f_act(f[:], f[:], AF.Sigmoid), reads=[K("f")], writes=[K("f")])
            s.op("dve", f_ts(f[:], f[:], oml[j][:], lb[j][:], ALU.mult, ALU.add), reads=[K("f"), ("oml", j), ("lb", j)], writes=[K("f")])
            s.op("act", f_act(t1[:], f[:], AF.Ln), reads=[K("f")], writes=[K("t1")])
            s.op("dve", f_scan(b[:], rst[:], t1[:], 0.0, ALU.mult, ALU.add), reads=[K("t1"), "rst"], writes=[K("b")])
            s.op("pool", f_ts(f[:], f[:], -1.0, 1.0, ALU.mult, ALU.add), reads=[K("f")], writes=[K("f")])
            b3 = b[:].rearrange("p (c t) -> p c t", t=CL)
            bl = b3[:, :, CL - 1]; br = b3[:, :, CL // 2 - 1]
            s.op("act", f_act(W["dec", j][:], bl, AF.Exp), reads=[K("b")], writes=[K("dec")])
            s.op("act", f_act(t1[:], b[:], AF.Exp), reads=[K("b"), K("t1")], writes=[K("t1")])
            s.op("dve", f_stt(W["Qh", j][:], q[:], sc, t1[:], ALU.mult, ALU.mult), reads=[K("q"), K("t1")], writes=[K("Qh")])
            t13 = t1[:].rearrange("p (c t) -> p c t", t=CL); t23 = t2[:].rearrange("p (c t) -> p c t", t=CL)
            s.op("dve", f_tt(t23, bc_chunk(bl, CL), b3, ALU.subtract), reads=[K("b"), K("t2")], writes=[K("t2")])
            s.op("act", f_act(t2[:], t2[:], AF.Exp), reads=[K("t2")], writes=[K("t2")])
            s.op("pool", f_tt(W["Kh", j][:], f[:], t2[:], ALU.mult), reads=[K("f"), K("t2")], writes=[K("Kh")])
            s.op("dve", f_tt(t13, b3, bc_chunk(br, CL), ALU.subtract), reads=[K("b"), K("t1"), K("Qh")], writes=[K("t1")])
            s.op("dve", f_ts(t2[:], t1[:], -1.0, 40.0, ALU.mult, ALU.min), reads=[K("t1"), K("Kh")], writes=[K("t2")])
            s.op("pool", f_ts(t1[:], t1[:], 40.0, None, ALU.min), reads=[K("t1")], writes=[K("t1")])
            s.op("act", f_act(t1[:], t1[:], AF.Exp), reads=[K("t1")], writes=[K("t1")])
            s.op("act", f_act(t2[:], t2[:], AF.Exp), reads=[K("t2")], writes=[K("t2")])
            s.op("dve", f_stt(W["Qt", j][:], q[:], sc, t1[:], ALU.mult, ALU.mult), reads=[K("q"), K("t1")], writes=[K("Qt")])
            s.op("pool", f_tt(W["Kt", j][:], f[:], t2[:], ALU.mult), reads=[K("f"), K("t2")], writes=[K("Kt")])
            keys.append({n: K(n) for n in ("Qt", "Kt", "Qh", "Kh", "dec", "V", "O")})
        la.run_segment([W["Qt", j] for j in range(2)], [W["Kt", j] for j in range(2)], [W["Qh", j] for j in range(2)],
                       [W["Kh", j] for j in range(2)], [W["dec", j] for j in range(2)], [W["V", j] for j in range(2)],
                       [W["O", j] for j in range(2)], keys)
        for j in range(2):
            la_backend(cx, s, "H", j, W["O", j], hg[j][t0:t0 + SEG, :].rearrange("(c p) e -> p c e", p=CL), nw_bc[:],
                       yh[j][t0:t0 + SEG, :].rearrange("(c p) e -> p c e", p=CL), 128, bufs, ("H", "O", j))

def emit_ret(cx, rq, rk, cos2, sin2, rv, rg, rnw, rtab, yr):
    s = cx.s; sb = cx.sb; nc = cx.nc; es = cx.es
    mask, ident = cx.consts["mask"], cx.consts["ident"]
    ps_a, ps_o, ps_t, ps_u = la_psum(cx, 256)
    la = LA(cx, "R", 2, 256, ps_a, ps_o, ps_t, ps_u, mask, ident)
    nw_bc = sb("r_nw", [CL, 256], F32)
    s.dma("sp", f_dma(nw_bc[:], rnw.partition_broadcast(CL)), "rnw", writes=["nwR"])
    tab = [sb("r_tab%d" % j, [128, 5, CL], F32) for j in range(2)]
    dec = [sb("r_dec%d" % j, [128, NCH], F32) for j in range(2)]
    for j in range(2):
        s.dma("sp", f_dma(tab[j][:], rtab[j]), "rtab%d" % j, writes=[("rtab", j)])
        s.op("dve", f_copy(dec[j][:].rearrange("p (a c) -> p a c", c=CL), bc_mid(tab[j][:, 4, :], NCH // CL)), reads=[("rtab", j)], writes=[("R", "dec", j)])
    cs = sb("r_cos", [128, SEG], F32); sn = sb("r_sin", [128, SEG], F32)
    W = {}
    for nm in ("a", "as", "qr", "kr"):
        W[nm, 0] = W[nm, 1] = sb("r_%s" % nm, [128, SEG], F32)
    for j in range(2):
        for nm in ("Qt", "Kt", "Qh", "Kh"):
            W[nm, j] = sb("r_%s%d" % (nm, j), [128, SEG], BF16)
        W["V", j] = sb("r_V%d" % j, [CL, NCH, 256], BF16)
        W["O", j] = sb("r_O%d" % j, [CL, NCH, 256], F32)
    bufs = (sb("r_gt", [CL, NCH, 256], F32), sb("r_ssq", [CL, NCH], F32), sb("r_rs", [CL, NCH], F32))
    for seg in range(S // SEG):
        t0 = seg * SEG
        s.dma("sp", f_dma(cs[:], cos2[:, t0:t0 + SEG]), "rcos", writes=["rcos"])
        s.dma("sp", f_dma(sn[:], sin2[:, t0:t0 + SEG]), "rsin", writes=["rsin"])
        keys = []
        for j in range(2):
            K = lambda n, j=j: ("R", n, j) if n in ("Qt", "Kt", "Qh", "Kh", "dec", "V", "O") else ("R", n)
            a, as_, qr, kr = (W[n, j] for n in ("a", "as", "qr", "kr"))
            for (src, dst, dk) in ((rq, qr, "qr"), (rk, kr, "kr")):
                s.dma("sp", f_dma(a[:], src[j][:, t0:t0 + SEG]), "ra%d" % j, writes=[K("a")])
                s.dma("sp", f_dma(as_[0:64, :], src[j][64:128, t0:t0 + SEG]), "ras%d" % j, writes=[K("as")])
                s.dma("sp", f_dma(as_[64:128, :], src[j][0:64, t0:t0 + SEG]), "rasb%d" % j, writes=[K("as")])
                s.op("dve", f_tt(a[:], a[:], cs[:], ALU.mult), reads=[K("a"), "rcos"], writes=[K("a")])
                s.op("pool", f_tt(as_[:], as_[:], sn[:], ALU.mult), reads=[K("as"), "rsin"], writes=[K("as")])
                s.op("dve", f_tt(dst[:], a[:], as_[:], ALU.add), reads=[K("a"), K("as")], writes=[K(dk)])
            s.dma("pool", f_dma(W["V", j][:], rv[j][t0:t0 + SEG, :].rearrange("(c p) e -> p c e", p=CL)), "rV%d" % j, writes=[K("V")])
            for ti, (nm, srcb, eng) in enumerate((("Qt", qr, "dve"), ("Kt", kr, "pool"), ("Qh", qr, "dve"), ("Kh", kr, "pool"))):
                s.op(eng, f_tt(W[nm, j][:].rearrange("p (c t) -> p c t", t=CL), srcb[:].rearrange("p (c t) -> p c t", t=CL),
                               bc_mid(tab[j][:, ti, :], NCH), ALU.mult), reads=[K("qr" if srcb is qr else "kr"), ("rtab", j)], writes=[K(nm)])
            keys.append({n: K(n) for n in ("Qt", "Kt", "Qh", "Kh", "dec", "V", "O")})
        la.run_segment([W["Qt", j] for j in range(2)], [W["Kt", j] for j in range(2)], [W["Qh", j] for j in range(2)],
                       [W["Kh", j] for j in range(2)], dec, [W["V", j] for j in range(2)], [W["O", j] for j in range(2)], keys)
        for j in range(2):
            la_backend(cx, s, "R", j, W["O", j], rg[j][t0:t0 + SEG, :].rearrange("(c p) e -> p c e", p=CL), nw_bc[:],
                       yr[j][t0:t0 + SEG, :].rearrange("(c p) e -> p c e", p=CL), 256, bufs, ("R", "O", j))

def load_consts(cx, mask_d, ident_d, rst_d):
    s = cx.s
    mask = cx.sb("c_mask", [CL, CL], F32); ident = cx.sb("c_ident", [128, 128], BF16); rst = cx.sb("c_rst", [128, SEG], F32)
    s.dma("sp", f_dma(mask[:], mask_d), "cmask", writes=["mask"])
    s.dma("pool", f_dma(ident[:], ident_d), "cident", writes=["ident"])
    s.dma("sp", f_dma(rst[:], rst_d), "crst", writes=["rst"])
    cx.consts = {"mask": mask, "ident": ident, "rst": rst}
    return cx.consts

def host_consts():
    mask = np.triu(np.ones((CL, CL), np.float32))
    ident = np.eye(128, dtype=np.float32)
    rst = np.ones((128, SEG), np.float32); rst[:, ::CL] = 0.0
    return mask, ident, rst

def host_ret_tables(head):
    lg = np.log(1.0 - 2.0 ** (-5.0 - head))
    t = np.arange(CL, dtype=np.float64)
    sc = 128 ** -0.5
    tab = np.stack([np.exp(lg * (t - 15)), sc * np.exp(lg * (15 - t)), np.exp(lg * (t + 1)), sc * np.exp(lg * (31 - t)),
                    np.full(CL, np.exp(lg * CL))], 0)
    return np.broadcast_to(tab[None], (128, 5, CL)).astype(np.float32).copy()

def host_rope_tables():
    half = 64
    inv = (1.0 / (10000.0 ** (np.arange(half, dtype=np.float32) / half))).astype(np.float32)
    ang = (np.arange(S, dtype=np.float32)[:, None] * inv[None, :]).astype(np.float32)
    cos, sin = np.cos(ang).astype(np.float32).T, np.sin(ang).astype(np.float32).T
    return np.concatenate([cos, cos], 0).copy(), np.concatenate([-sin, sin], 0).copy()

QS = 256; NQS = S // QS; NKB = S // 128

def host_diff_consts():
    k = np.arange(128)[:, None]; q = np.arange(128)[None, :]
    return np.concatenate([(q - k), (128 + q - k)], 1).astype(np.float32)

def t5_steps():
    n = np.arange(0, 256)
    nf = np.maximum(n, 1).astype(np.float32)
    large = 16 + (np.log(nf / np.float32(16)) / np.float32(np.log(128 / 16)) * np.float32(16)).astype(np.int32)
    large = np.minimum(large, 31)
    b = np.where(n < 16, n, large)
    steps = [(int(i), int(b[i]), int(b[i - 1])) for i in range(1, 256) if b[i] != b[i - 1]]
    return int(b[0]), steps

def emit_diff(cx, layer, dq, dk, dv_, relb, dlam, dnw, dmat, yd, stage=1):
    import math
    s = cx.s; sb = cx.sb
    lam_init = 0.8 - 0.6 * math.exp(-0.3 * layer)
    pb = cx.pb
    dl = sb("d_dl", [128, 256], F32); pr = sb("d_pr", [128, 128], F32); s12 = sb("d_s12", [128, 2], F32); nlam = sb("d_nlam", [128, 1], F32)
    s.dma("sp", f_dma(dl[:], dlam.partition_broadcast(128)), "ddl", writes=["dl"])
    dl3 = dl[:].rearrange("p (a d) -> p a d", d=64)
    s.op("dve", f_tt(pr[:].rearrange("p (a d) -> p a d", d=64), dl3[:, 0::2, :], dl3[:, 1::2, :], ALU.mult), reads=["dl"], writes=["pr"])
    s.op("dve", f_reduce(s12[:], pr[:].rearrange("p (a d) -> p a d", d=64), AX.X, ALU.add), reads=["pr"], writes=["s12"])
    s.op("act", f_act(s12[:], s12[:], AF.Exp), reads=["s12"], writes=["s12"])
    s.op("dve", f_tt(nlam[:], s12[:, 1:2], s12[:, 0:1], ALU.subtract), reads=["s12"], writes=["nlam"])
    s.op("dve", f_ts(nlam[:], nlam[:], -lam_init, None, ALU.add), reads=["nlam"], writes=["nlam"])
    nw = sb("d_nw", [128, 128], F32)
    s.dma("sp", f_dma(nw[:], dnw.partition_broadcast(128)), "dnw", writes=["dnw"])
    s.op("dve", f_ts(nw[:], nw[:], 1.0 - lam_init, None, ALU.mult), reads=["dnw"], writes=["dnw"])
    dm = sb("d_dm", [128, 256], F32)
    s.dma("sp", f_dma(dm[:], dmat), "ddm", writes=["dm"])
    b0, steps = t5_steps()
    E = []
    for j in range(2):
        rb = sb("d_rb%d" % j, [128, 32], F32); acc = sb("d_acc%d" % j, [128, 256], F32); tmp = sb("d_tmp%d" % j, [128, 256], F32)
        dlt = sb("d_dlt%d" % j, [128, 1], F32); Ej = sb("d_E%d" % j, [128, 256], BF16)
        s.dma("sp", f_dma(rb[:], relb[j].partition_broadcast(128)), "drb%d" % j, writes=[("rb", j)])
        s.op("dve", f_tt(dlt[:], rb[:, b0:b0 + 1], rb[:, 31:32], ALU.subtract), reads=[("rb", j)], writes=[("dlt", j)])
        s.op("dve", f_ts(acc[:], dm[:], 0.0, dlt[:], ALU.mult, ALU.add), reads=["dm", ("dlt", j)], writes=[("acc", j)])
        for (n, bn, bo) in steps:
            s.op("dve", f_tt(dlt[:], rb[:, bn:bn + 1], rb[:, bo:bo + 1], ALU.subtract), reads=[("rb", j), ("dlt", j)], writes=[("dlt", j)])
            s.op("dve", f_ts(tmp[:], dm[:], float(n), dlt[:], ALU.is_ge, ALU.mult), reads=["dm", ("dlt", j)], writes=[("tmp", j)])
            s.op("dve", f_tt(acc[:], acc[:], tmp[:], ALU.add), reads=[("acc", j), ("tmp", j)], writes=[("acc", j)])
        s.op("act", f_act(acc[:], acc[:], AF.Exp), reads=[("acc", j)], writes=[("acc", j)])
        s.op("dve", f_ts(tmp[:], dm[:], 0.0, None, ALU.is_ge), reads=["dm"], writes=[("tmp", j)])
        s.op("dve", f_tt(Ej[:], acc[:], tmp[:], ALU.mult), reads=[("acc", j), ("tmp", j)], writes=[("E", j)])
        E.append(Ej)
    if stage == 0:
        dbg = sb("d_dbg", [128, 256], F32)
        s.op("dve", f_copy(dbg[:], E[0][:]), reads=[("E", 0)], writes=["dbg"])
        s.dma("sp", f_dma(yd[0][0:128, :], dbg[:, 0:128]), "dbg0", reads=["dbg"])
        s.dma("sp", f_dma(yd[0][128:256, :], dbg[:, 128:256]), "dbg1", reads=["dbg"])
        s.op("dve", f_ts(dbg[:, 0:128], dm[:, 0:128], 0.0, nlam[:], ALU.mult, ALU.add), reads=["dbg", "nlam", "dm"], writes=["dbg2"]); s.dma("sp", f_dma(yd[1][0:128, :], dbg[:, 0:128]), "dbg2", reads=["dbg2"])
        s.dma("sp", f_dma(yd[1][128:256, :], nw[:]), "dbg3", reads=["dnw"])
        return
    QTa = sb("d_QTa", [128, S], BF16); QTb = sb("d_QTb", [128, S], BF16); KT = sb("d_KT", [128, S], BF16); VX = sb("d_VX", [128, NKB, 132], BF16)
    P = [sb("d_P%d" % i, [128, 512], BF16) for i in range(3)]
    ut = [sb("d_u%d" % i, [128, 128], F32) for i in range(2)]; tt_ = [sb("d_t%d" % i, [128, 128], F32) for i in range(2)]
    yt = [sb("d_y%d" % i, [128, 128], F32) for i in range(2)]; junk = sb("d_junk", [128, 128], F32)
    sm = [sb("d_sm%d" % i, [128, 4], F32) for i in range(2)]
    ps_s = [pb[0], pb[1], pb[2]]
    ps_o = [[pb[3][:, 0:129], pb[4][:, 0:129]], [pb[5][:, 0:129], pb[6][:, 0:129]]]
    s.op("dve", f_memset(VX[:, :, 128:129], 1.0), writes=["VXone"])
    s.op("dve", f_memset(QTa[64:128, :], 0.0), writes=["QTz"])
    s.op("pool", f_memset(QTb[0:64, :], 0.0), writes=["QTz2"])
    it = 0; fin = 0
    for j in range(2):
        for q4 in range(4):
            sl = slice(q4 * (S // 4), (q4 + 1) * (S // 4))
            s.dma("pool", f_dma(QTa[0:64, sl], dq[j][0:64, sl]), "dQT", writes=["QT"])
            s.dma("pool", f_dma(QTb[64:128, sl], dq[j][64:128, sl]), "dQT2", writes=["QT"])
            s.dma("pool", f_dma(KT[:, sl], dk[j][:, sl]), "dKT", writes=["KT"])
        s.dma("pool", f_dma(VX[:, :, 0:128], dv_[j].rearrange("(kb p) e -> p kb e", p=128)), "dVX", writes=["VX"])
        for qs in range(NQS):
            qb0 = 2 * qs; qb1 = qb0 + 1
            qsl = slice(qs * QS, (qs + 1) * QS)
            for kb in range(qb1 + 1):
                ksl = slice(kb * 128, (kb + 1) * 128)
                i = it % 3; it += 1
                pss = ps_s[i]; Pt = P[i]
                s.group("pe", [f_mm(pss[:, 0:256], KT[:, ksl], QTa[:, qsl], True, True),
                               f_mm(pss[:, 256:512], KT[:, ksl], QTb[:, qsl], True, True)],
                        reads=["QT", "KT", "QTz", "QTz2"], writes=[("pss", i)])
                s.op("act", f_act(Pt[:], pss[:], AF.Exp, scale=0.125), reads=[("pss", i)], writes=[("P", i)])
                P3 = Pt[:].rearrange("p (m q) -> p m q", m=2)
                def mul(qoff, eoff):
                    s.op("dve", f_tt(P3[:, :, qoff:qoff + 128], P3[:, :, qoff:qoff + 128], bc_mid(E[j][:, eoff:eoff + 128], 2), ALU.mult),
                         reads=[("P", i), ("E", j)], writes=[("P", i)])
                jqs = [0, 1]
                if kb == qb0 - 1: mul(0, 128)
                if kb == qb0: mul(0, 0); mul(128, 128)
                if kb == qb1: mul(128, 0); jqs = [1]
                fns = []; wr = []
                for jq in jqs:
                    last = qb0 if jq == 0 else qb1
                    for m in range(2):
                        fns.append(f_mm(ps_o[jq][m], Pt[:, m * 256 + jq * 128:m * 256 + jq * 128 + 128], VX[:, kb, 0:129], kb == 0, kb == last))
                        wr.append(("pso", jq, m))
                s.group("pe", fns, reads=[("P", i), "VX", "VXone"], writes=wr)
                for jq in jqs:
                    last = qb0 if jq == 0 else qb1
                    if kb != last: continue
                    f = fin % 2; fin += 1
                    O1, O2 = ps_o[jq][0], ps_o[jq][1]
                    k1, k2 = ("pso", jq, 0), ("pso", jq, 1)
                    s.op("dve", f_recip(sm[f][:, 0:1], O1[:, 128:129]), reads=[k1], writes=[("sm", f)])
                    s.op("dve", f_recip(sm[f][:, 1:2], O2[:, 128:129]), reads=[k2, ("sm", f)], writes=[("sm", f)])
                    s.op("dve", f_tt(sm[f][:, 1:2], sm[f][:, 1:2], nlam[:], ALU.mult), reads=[("sm", f), "nlam"], writes=[("sm", f)])
                    s.op("act", f_act(tt_[f][:], O1[:, 0:128], AF.Copy, scale=sm[f][:, 0:1]), reads=[k1, ("sm", f)], writes=[("tt", f)])
                    s.op("dve", f_stt(ut[f][:], O2[:, 0:128], sm[f][:, 1:2], tt_[f][:], ALU.mult, ALU.add), reads=[k2, ("sm", f), ("tt", f)], writes=[("ut", f)])
                    s.op("act", f_act(junk[:], ut[f][:], AF.Square), reads=[("ut", f)], writes=["junk"])
                    s.op("dve", f_reduce(sm[f][:, 2:3], junk[:], AX.X, ALU.add), reads=["junk", ("sm", f)], writes=[("sm", f)])
                    s.op("act", f_act(sm[f][:, 3:4], sm[f][:, 2:3], AF.Sqrt, bias=EPS, scale=1.0 / 128), reads=[("sm", f)], writes=[("sm", f)])
                    s.op("dve", f_recip(sm[f][:, 3:4], sm[f][:, 3:4]), reads=[("sm", f)], writes=[("sm", f)])
                    s.op("dve", f_stt(yt[f][:], ut[f][:], sm[f][:, 3:4], nw[:], ALU.mult, ALU.mult), reads=[("ut", f), ("sm", f), "dnw"], writes=[("yt", f)])
                    r0 = (qb0 + jq) * 128
                    s.dma("sp", f_dma(yd[j][r0:r0 + 128, :], yt[f][:]), "dy%d" % f, reads=[("yt", f)])


DEPTH = 4
_PROGS = {}

def _prog_a():
    if "A" not in _PROGS:
        _PROGS["A"] = build_phase_a(NIN)
    return _PROGS["A"]

def _prog_c(last):
    k = ("C", bool(last))
    if k not in _PROGS:
        _PROGS[k] = build_phase_c(bool(last))
    return _PROGS[k]

def _prog_b(layer):
    k = ("B", layer)
    if k in _PROGS:
        return _PROGS[k]
    nc = bass.Bass("TRN2", target_bir_lowering=False)
    di = lambda name, shape: nc.dram_tensor(name, shape, F32, kind="ExternalInput").ap()
    do = lambda name, shape: nc.dram_tensor(name, shape, F32, kind="ExternalOutput").ap()
    mask_d = di("mask", [CL, CL]); ident_d = di("ident", [128, 128]); rst_d = di("rst", [128, SEG])
    hq = di("hq", [2, 128, S]); hf = di("hf", [2, 128, S]); hi = di("hi", [2, S, 128]); hg = di("hg", [2, S, 128])
    lbl = di("lbl", [2, 128, 4]); hnw = di("hnw", [128]); yh = do("yh", [2, S, 128])
    rq = di("rq", [2, 128, S]); rk = di("rk", [2, 128, S])
    cos2 = di("cos2", [128, S]); sin2 = di("sin2", [128, S]); rv = di("rv", [2, S, 256]); rg = di("rg", [2, S, 256])
    rnw = di("rnw", [256]); rtab = di("rtab", [2, 128, 5, CL]); yr = do("yr", [2, S, 256])
    dq = di("dq", [2, 128, S]); dk = di("dk", [2, 128, S]); dv_ = di("dv", [2, S, 128]); relb = di("relb", [2, 32])
    dlam = di("dlam", [256]); dnw = di("dnw", [128]); dmat = di("dmat", [128, 256]); yd = do("yd", [2, S, 128])
    with contextlib.ExitStack() as es0:
        s = Sched(nc, es0)
        with contextlib.ExitStack() as es:
            cx = Ctx(nc, es, s, gemm=False); alloc_b_psum(cx)
            load_consts(cx, mask_d, ident_d, rst_d)
            with contextlib.ExitStack() as es2:
                cx.es = es2
                emit_hgrn(cx, layer, hq, hf, hi, hg, lbl, hnw, yh, cx.consts)
                sched_barrier(s); sched_flush(s)
            with contextlib.ExitStack() as es2:
                cx.es = es2
                emit_ret(cx, rq, rk, cos2, sin2, rv, rg, rnw, rtab, yr)
                sched_barrier(s); sched_flush(s)
            with contextlib.ExitStack() as es2:
                cx.es = es2
                emit_diff(cx, layer, dq, dk, dv_, relb, dlam, dnw, dmat, yd)
                sched_barrier(s); sched_flush(s)
            s.finish(); sched_flush(s)
    _PROGS[k] = nc
    return nc

_OFF = {"hq": 0, "hf": 1024, "hi": 2048, "hg": 3072, "dq": 4096, "dk": 5120, "dv": 6144,
        "rq": 7168, "rk": 8192, "rv": 9216, "rg": 11264, "gd": 13312}

def _lay(v):
    return np.ascontiguousarray(np.asarray(v, np.float32).reshape(-1, 128).T)

def kernel(x, attn_norm_w, w_in, lb_logits, hgrn_norm_w, rel_bias, diff_lambda, diff_norm_w, ret_norm_w, w_gate_up,
           b_gate, w_br_hgrn, w_br_diff, w_br_ret, w_o, ffn_norm_w, w_ffn_gate, w_ffn_up, w_ffn_down, final_norm_w):
    f32 = lambda a: np.asarray(a, np.float32)
    x = f32(x)
    NCORE = 8; cores = list(range(NCORE))
    xT = [np.ascontiguousarray(x[c // 4, (c % 4) * TOK:(c % 4 + 1) * TOK, :].T) for c in cores]
    mask, ident, rst = host_consts()
    cos2, sin2 = host_rope_tables()
    dmat = host_diff_consts()
    rtabs = [host_ret_tables(h) for h in range(8)]
    lbl_all = f32(lb_logits)
    for l in range(DEPTH):
        wl = f32(w_in[l]); anw = _lay(attn_norm_w[l])
        res = run_bass_kernel_spmd(_prog_a(), [{"xT": xT[c], "w": wl, "anw": anw} for c in cores], core_ids=cores)
        projT = [res.results[c]["projT"] for c in cores]
        del res
        PT = [np.concatenate(projT[b * 4:(b + 1) * 4], axis=1) for b in range(2)]
        gdT = [np.ascontiguousarray(projT[c][_OFF["gd"]:_OFF["gd"] + GR, :]) for c in cores]
        del projT
        in_b = []
        for c in cores:
            b, hp = c // 4, c % 4
            P = PT[b]
            fm = lambda name, d: np.ascontiguousarray(P[_OFF[name] + 2 * hp * d:_OFF[name] + 2 * (hp + 1) * d, :].reshape(2, d, S))
            tm = lambda name, d: np.ascontiguousarray(P[_OFF[name] + 2 * hp * d:_OFF[name] + 2 * (hp + 1) * d, :].reshape(2, d, S).transpose(0, 2, 1))
            in_b.append({
                "mask": mask, "ident": ident, "rst": rst,
                "hq": fm("hq", 128), "hf": fm("hf", 128), "hi": tm("hi", 128), "hg": tm("hg", 128),
                "lbl": np.ascontiguousarray(lbl_all[:, 2 * hp * 128:2 * (hp + 1) * 128].T.reshape(2, 128, 4)),
                "hnw": f32(hgrn_norm_w[l]),
                "rq": fm("rq", 128), "rk": fm("rk", 128), "cos2": cos2, "sin2": sin2,
                "rv": tm("rv", 256), "rg": tm("rg", 256), "rnw": f32(ret_norm_w[l]),
                "rtab": np.stack([rtabs[2 * hp], rtabs[2 * hp + 1]], 0),
                "dq": fm("dq", 128), "dk": fm("dk", 128), "dv": tm("dv", 128),
                "relb": np.ascontiguousarray(f32(rel_bias)[:, 2 * hp:2 * hp + 2].T),
                "dlam": np.ascontiguousarray(f32(diff_lambda[l]).reshape(256)), "dnw": f32(diff_norm_w[l]), "dmat": dmat,
            })
        del PT
        res = run_bass_kernel_spmd(_prog_b(l), in_b, core_ids=cores)
        del in_b
        yT = []
        Y = [np.empty((S, D), np.float32) for _ in range(2)]
        for c in cores:
            b, hp = c // 4, c % 4
            r = res.results[c]
            for j in range(2):
                h = 2 * hp + j
                Y[b][:, h * 128:(h + 1) * 128] = r["yh"][j]
                Y[b][:, 1024 + h * 128:1024 + (h + 1) * 128] = r["yd"][j]
                Y[b][:, 2048 + h * 256:2048 + (h + 1) * 256] = r["yr"][j]
        del res
        for c in cores:
            yT.append(np.ascontiguousarray(Y[c // 4][(c % 4) * TOK:(c % 4 + 1) * TOK, :].T))
        del Y
        last = (l == DEPTH - 1)
        wts = {"wgu": f32(w_gate_up[l]), "bg": _lay(b_gate[l]), "wbh": f32(w_br_hgrn[l]), "wbd": f32(w_br_diff[l]),
               "wbr": f32(w_br_ret[l]), "wo": f32(w_o[l]), "fnw": _lay(ffn_norm_w[l]), "wg": f32(w_ffn_gate[l]),
               "wu": f32(w_ffn_up[l]), "wd": f32(w_ffn_down[l]), "lnw": _lay(final_norm_w)}
        res = run_bass_kernel_spmd(_prog_c(last), [dict(wts, yT=yT[c], gdT=gdT[c], xT=xT[c]) for c in cores], core_ids=cores)
        xT = [res.results[c]["outT"] for c in cores]
        del res, yT, gdT, wts
    out = np.empty((2, S, D), np.float32)
    for c in cores:
        out[c // 4, (c % 4) * TOK:(c % 4 + 1) * TOK, :] = xT[c].T
    return out
```

```python
import contextlib, numpy as np
import concourse.bass as bass
import concourse.mybir as mybir
from concourse.bass_utils import run_bass_kernel_spmd
F32 = mybir.dt.float32; BF16 = mybir.dt.bfloat16
ALU = mybir.AluOpType; AF = mybir.ActivationFunctionType; AX = mybir.AxisListType

class Sched:
    ENGS = ("pe", "act", "dve", "pool", "sp")
    def __init__(self, nc, es, n_dma_sems=40):
        self.nc = nc; self.es = es
        self.streams = {e: [] for e in self.ENGS}
        self.esem = {e: es.enter_context(nc.semaphore("es_" + e)) for e in ("pe", "act", "dve", "pool")}
        self.ecnt = {e: 0 for e in self.esem}
        self.waited = {e: {} for e in self.ENGS}
        self.lastw = {}; self.reads = {}
        self.dsem = {}; self.dcnt = {}
        self.n_dma = 0
    def _dma_sem(self, key):
        if key not in self.dsem:
            self.dsem[key] = self.es.enter_context(self.nc.semaphore("ds_%d" % len(self.dsem)))
            self.dcnt[key] = 0
        return self.dsem[key]
    def _deps(self, eng, reads, writes):
        deps = []
        for b in list(reads) + list(writes):
            if b in self.lastw: deps.append(self.lastw[b])
        for b in writes:
            deps.extend(self.reads.get(b, ()))
        need = {}
        for (sem, val) in deps:
            k = id(sem)
            if self.waited[eng].get(k, 0) >= val: continue
            if k not in need or need[k][1] < val: need[k] = (sem, val)
        for k, (sem, val) in need.items():
            self.waited[eng][k] = val
            self.streams[eng].append(("wait", sem, val))
    def _commit(self, tick, reads, writes):
        for b in writes:
            self.lastw[b] = tick; self.reads[b] = []
        for b in reads:
            self.reads.setdefault(b, []).append(tick)
    def op(self, eng, fn, reads=(), writes=()):
        self._deps(eng, reads, writes)
        self.ecnt[eng] += 1
        tick = (self.esem[eng], self.ecnt[eng])
        self.streams[eng].append(("op", fn, self.esem[eng], 1))
        self.waited[eng][id(self.esem[eng])] = max(self.waited[eng].get(id(self.esem[eng]), 0), 0)
        self._commit(tick, reads, writes)
    def group(self, eng, fns, reads=(), writes=()):
        self._deps(eng, reads, writes)
        for f in fns[:-1]:
            self.streams[eng].append(("op", f, None, 0))
        self.ecnt[eng] += 1
        tick = (self.esem[eng], self.ecnt[eng])
        self.streams[eng].append(("op", fns[-1], self.esem[eng], 1))
        self._commit(tick, reads, writes)
    def dma(self, eng, fn, semkey, reads=(), writes=()):
        sem = self._dma_sem(semkey)
        self._deps(eng, reads, writes)
        self.dcnt[semkey] += 16
        tick = (sem, self.dcnt[semkey])
        self.streams[eng].append(("op", fn, sem, 16))
        self._commit(tick, reads, writes)
    def finish(self):
        for key, sem in self.dsem.items():
            if self.dcnt[key]: self.streams["sp"].append(("wait", sem, self.dcnt[key]))
        for e, sem in self.esem.items():
            if self.ecnt[e]: self.streams["sp"].append(("wait", sem, self.ecnt[e]))
    def replay(self):
        nc = self.nc
        def run(engobj, items):
            for it in items:
                if it[0] == "wait":
                    engobj.wait_ge(it[1], it[2])
                else:
                    ins = it[1](engobj)
                    if it[2] is not None:
                        ins.then_inc(it[2], it[3])
        with nc.Block() as block:
            @block.sync
            def _(e): run(e, self.streams["sp"])
            @block.tensor
            def _(e): run(e, self.streams["pe"])
            @block.scalar
            def _(e): run(e, self.streams["act"])
            @block.vector
            def _(e): run(e, self.streams["dve"])
            @block.gpsimd
            def _(e): run(e, self.streams["pool"])
    def stats(self):
        return {e: len(v) for e, v in self.streams.items()}

def sched_barrier(s):
    for eng in s.ENGS:
        for e2, sem in s.esem.items():
            v = s.ecnt[e2]
            if v and s.waited[eng].get(id(sem), 0) < v:
                s.streams[eng].append(("wait", sem, v)); s.waited[eng][id(sem)] = v
        for key, sem in s.dsem.items():
            v = s.dcnt[key]
            if v and s.waited[eng].get(id(sem), 0) < v:
                s.streams[eng].append(("wait", sem, v)); s.waited[eng][id(sem)] = v
    s.lastw = {}; s.reads = {}

def sched_flush(s):
    s.replay()
    for e in s.streams: s.streams[e] = []


D = 4096; KC = 32; TOK = 2048; TS = 512; NIN = 13568; FFN = 11008; GR = 256
EPS = 1e-6
WMAX = 16384

def f_mm(out, lhsT, rhs, start, stop):
    return lambda e: e.matmul(out, lhsT=lhsT, rhs=rhs, start=start, stop=stop)
def f_dma(out, in_):
    return lambda e: e.dma_start(out=out, in_=in_)
def f_act(out, in_, func, bias=None, scale=None):
    kw = {}
    if bias is not None: kw["bias"] = bias
    if scale is not None: kw["scale"] = scale
    return lambda e: e.activation(out=out, in_=in_, func=func, **kw)
def f_tt(out, in0, in1, op):
    return lambda e: e.tensor_tensor(out=out, in0=in0, in1=in1, op=op)
def f_ts(out, in0, s1, s2, op0, op1=None):
    if op1 is None:
        return lambda e: e.tensor_scalar(out=out, in0=in0, scalar1=s1, scalar2=None, op0=op0)
    return lambda e: e.tensor_scalar(out=out, in0=in0, scalar1=s1, scalar2=s2, op0=op0, op1=op1)
def f_stt(out, in0, scalar, in1, op0, op1):
    return lambda e: e.scalar_tensor_tensor(out=out, in0=in0, scalar=scalar, in1=in1, op0=op0, op1=op1)
def f_copy(out, in_):
    return lambda e: e.tensor_copy(out=out, in_=in_)
def f_recip(out, in_):
    return lambda e: e.reciprocal(out=out, in_=in_)
def f_memset(ap, v):
    return lambda e: e.memset(ap, v)

class Ctx:
    def __init__(self, nc, es, s, nps=7, gemm=True):
        self.nc = nc; self.es = es; self.s = s
        self.nps = nps; self.rr = 0
        if gemm:
            self.ps = [es.enter_context(nc.psum_tensor("ps%d" % i, [128, 512], F32)) for i in range(8)]
            self.wb = [es.enter_context(nc.sbuf_tensor("wb%d" % i, [128, WMAX], BF16)) for i in range(2)]
        self.wcount = 0
        self.ones = es.enter_context(nc.sbuf_tensor("ones", [128, 128], BF16))
        s.op("dve", f_memset(self.ones[:], 1.0), writes=["ones"])
        self._n = 0
    def sb(self, name, shape, dt):
        return self.es.enter_context(self.nc.sbuf_tensor(name, shape, dt))
    def next_ps(self):
        i = self.rr % self.nps; self.rr += 1
        return self.ps[i], ("ps", i)

def gemm(cx, groups, ncols, CB, epilogue, fo_base=0):
    s = cx.s
    ncb = (ncols + CB - 1) // CB
    secoff = []; off = 0
    for g in groups:
        secoff.append(off); off += g["nk"] * CB
    assert off <= WMAX, off
    def issue_w(cb):
        c0 = cb * CB; cw = min(CB, ncols - c0)
        slot = cx.wcount % 2; cx.wcount += 1
        for gi, g in enumerate(groups):
            sec = cx.wb[slot][:, secoff[gi]:secoff[gi] + g["nk"] * CB].rearrange("p (k c) -> p k c", c=CB)
            src = g["wap"](c0, cw).rearrange("(k p) n -> p k n", p=128)
            s.dma("pool", f_dma(sec[:, :, 0:cw], src), "wb%d_%d" % (slot, gi), writes=[("wb", slot, gi)])
        return slot
    slots = {}
    slots[0] = issue_w(0)
    if ncb > 1: slots[1] = issue_w(1)
    for cb in range(ncb):
        c0 = cb * CB; cw = min(CB, ncols - c0)
        slot = slots[cb]
        for fi in range(cw // 128):
            fo = fo_base + (c0 // 128) + fi
            outs = []
            for gi, g in enumerate(groups):
                sec = cx.wb[slot][:, secoff[gi]:secoff[gi] + g["nk"] * CB].rearrange("p (k c) -> p k c", c=CB)
                pt, pk = cx.next_ps()
                fns = [f_mm(pt[:], sec[:, kc, fi * 128:(fi + 1) * 128], g["rhs"](kc), kc == 0, kc == g["nk"] - 1)
                       for kc in range(g["nk"])]
                s.group("pe", fns, reads=[("wb", slot, gi)] + list(g["rkeys"]), writes=[pk])
                outs.append((pt, pk))
            epilogue(fo, outs)
        if cb + 2 < ncb:
            slots[cb + 2] = issue_w(cb + 2)

def emit_norm_prologue(cx, xsrc, t0, nwsb, hT, rstd, tag, x_from_sbuf=None):
    s = cx.s
    xTv = xsrc.rearrange("(kc p) t -> p kc t", p=128)
    pss = cx.ps[7]
    for kc in range(KC):
        xb = cx.xs[kc % 3]; sqb = cx.sq[kc % 2]
        s.dma("sp", f_dma(xb[:], xTv[:, kc, t0:t0 + TS]), "xs%d" % (kc % 3), writes=[("xs", kc % 3)])
        s.op("act", f_act(sqb[:], xb[:], AF.Square), reads=[("xs", kc % 3)], writes=[("sq", kc % 2)])
        s.op("dve", f_ts(hT[:, kc, :], xb[:], nwsb[:, kc:kc + 1], None, ALU.mult),
             reads=[("xs", kc % 3), "nw" + tag], writes=[("hT", kc)])
        s.group("pe", [f_mm(pss[:], cx.ones[:], sqb[:], kc == 0, kc == KC - 1)], reads=[("sq", kc % 2), "ones"], writes=[("ps", 7)])
    s.op("act", f_act(rstd[:], pss[:], AF.Sqrt, bias=EPS, scale=1.0 / D), reads=[("ps", 7)], writes=["rstd"])
    s.op("dve", f_recip(rstd[:], rstd[:]), reads=["rstd"], writes=["rstd"])

_BF_RANGES = ((2048, 3072), (4096, 7168), (9216, 11264), (13312, 13568))
def _row_map():
    m = {}; nf = 0; nb = 0
    for fo in range(NIN // 128):
        r = fo * 128
        if any(a <= r < b for a, b in _BF_RANGES):
            m[fo] = (True, nb); nb += 128
        else:
            m[fo] = (False, nf); nf += 128
    return m, nf, nb
_ROWMAP, NROW_F, NROW_B = _row_map()

def emit_phase_a(cx, xT, w, anw, out, outb):
    s = cx.s
    hT = cx.sb("a_hT", [128, KC, TS], BF16)
    cx.xs = [cx.sb("a_xs%d" % i, [128, TS], F32) for i in range(3)]
    cx.sq = [cx.sb("a_sq%d" % i, [128, TS], BF16) for i in range(2)]
    ob = [cx.sb("a_ob%d" % i, [128, TS], F32) for i in range(3)]
    obh = [cx.sb("a_obh%d" % i, [128, TS], BF16) for i in range(3)]
    rstd = cx.sb("a_rstd", [128, TS], F32)
    anw_sb = cx.sb("a_anw", [128, KC], F32)
    s.dma("sp", f_dma(anw_sb[:], anw), "anw", writes=["nwA"])
    cnt = [0]
    for st in range(TOK // TS):
        t0 = st * TS
        emit_norm_prologue(cx, xT, t0, anw_sb, hT, rstd, "A")
        def epi(fo, outs, t0=t0):
            pt, pk = outs[0]
            i = cnt[0] % 3; cnt[0] += 1
            isb, r0 = _ROWMAP[fo]
            if isb:
                s.op("dve", f_tt(obh[i][:], pt[:], rstd[:], ALU.mult), reads=[pk, "rstd"], writes=[("obh", i)])
                s.dma("sp", f_dma(outb[r0:r0 + 128, t0:t0 + TS], obh[i][:]), "obh%d" % i, reads=[("obh", i)])
            else:
                s.op("dve", f_tt(ob[i][:], pt[:], rstd[:], ALU.mult), reads=[pk, "rstd"], writes=[("ob", i)])
                s.dma("sp", f_dma(out[r0:r0 + 128, t0:t0 + TS], ob[i][:]), "ob%d" % i, reads=[("ob", i)])
        grp = dict(nk=KC, rhs=lambda kc: hT[:, kc, :], rkeys=[("hT", kc) for kc in range(KC)],
                   wap=lambda c0, cw: w[:, c0:c0 + cw])
        gemm(cx, [grp], NIN, 512, epi)

def build_phase_a():
    nc = bass.Bass("TRN2", target_bir_lowering=False)
    xT = nc.dram_tensor("xT", [D, TOK], F32, kind="ExternalInput").ap()
    w = nc.dram_tensor("w", [D, NIN], F32, kind="ExternalInput").ap()
    anw = nc.dram_tensor("anw", [128, KC], F32, kind="ExternalInput").ap()
    out = nc.dram_tensor("projT", [NROW_F, TOK], F32, kind="ExternalOutput").ap()
    outb = nc.dram_tensor("projTb", [NROW_B, TOK], BF16, kind="ExternalOutput").ap()
    with contextlib.ExitStack() as es:
        s = Sched(nc, es)
        cx = Ctx(nc, es, s)
        emit_phase_a(cx, xT, w, anw, out, outb)
        s.finish(); s.replay()
    return nc

HH = FFN // 2
NKH = HH // 128

def emit_phase_c(cx, yT, gdT, xT, wgu, bg, wbh, wbd, wbr, wo, fnw, wg, wu, wd, lnw, outT, last):
    s = cx.s
    bufA = cx.sb("bufA", [128, 32, TS], BF16)
    bufB = cx.sb("bufB", [128, NKH, TS], BF16)
    gdb = cx.sb("gdb", [128, 2, TS], BF16)
    cx.xs = [cx.sb("xs%d" % i, [128, TS], F32) for i in range(3)]
    cx.sq = [cx.sb("sq%d" % i, [128, TS], BF16) for i in range(2)]
    gt = [[cx.sb("gt%d_%d" % (i, j), [128, TS], F32) for j in range(3)] for i in range(2)]
    mt = [cx.sb("mt%d" % i, [128, TS], F32) for i in range(2)]
    tt = [cx.sb("tt%d" % i, [128, TS], F32) for i in range(2)]
    x1t = [cx.sb("x1t%d" % i, [128, TS], F32) for i in range(3)]
    tg = [cx.sb("tg%d" % i, [128, TS], F32) for i in range(2)]
    tsg = [cx.sb("tsg%d" % i, [128, TS], F32) for i in range(2)]
    tu = [cx.sb("tu%d" % i, [128, TS], F32) for i in range(2)]
    rstd = cx.sb("c_rstd", [128, TS], F32)
    bg_sb = cx.sb("bg_sb", [128, 96], F32)
    fnw_sb = cx.sb("fnw_sb", [128, KC], F32)
    lnw_sb = cx.sb("lnw_sb", [128, KC], F32)
    s.dma("sp", f_dma(bg_sb[:], bg), "bg", writes=["bg"])
    s.dma("sp", f_dma(fnw_sb[:], fnw), "fnw", writes=["fnw"])
    if last:
        s.dma("sp", f_dma(lnw_sb[:], lnw), "lnw", writes=["lnw"])
    yTv = yT.rearrange("(kc p) t -> p kc t", p=128)
    gdv = gdT.rearrange("(kc p) t -> p kc t", p=128)
    pss = cx.ps[7]
    cnt = {"x": 0, "p3": 0}
    for st in range(TOK // TS):
        t0 = st * TS
        for q in range(4):
            s.dma("pool", f_dma(bufA[:, q * 8:(q + 1) * 8, :], yTv[:, q * 8:(q + 1) * 8, t0:t0 + TS]), "yin%d" % q,
                  writes=[("bufA", kc) for kc in range(q * 8, (q + 1) * 8)])
        s.dma("pool", f_dma(gdb[:], gdv[:, :, t0:t0 + TS]), "gdin", writes=["gdb"])
        def epi1(fo, outs):
            i = fo % 2
            for br in range(3):
                pt, pk = outs[br]
                s.op("act", f_act(gt[i][br][:], pt[:], AF.Sigmoid, bias=bg_sb[:, br * 32 + fo:br * 32 + fo + 1]),
                     reads=[pk, "bg"], writes=[("gt", i, br)])
            s.op("dve", f_tt(mt[i][:], outs[3][0][:], gt[i][0][:], ALU.mult), reads=[outs[3][1], ("gt", i, 0)], writes=[("mt", i)])
            s.op("dve", f_tt(tt[i][:], outs[4][0][:], gt[i][1][:], ALU.mult), reads=[outs[4][1], ("gt", i, 1)], writes=[("tt", i)])
            s.op("pool", f_tt(mt[i][:], mt[i][:], tt[i][:], ALU.add), reads=[("mt", i), ("tt", i)], writes=[("mt", i)])
            s.op("dve", f_tt(tt[i][:], outs[5][0][:], gt[i][2][:], ALU.mult), reads=[outs[5][1], ("gt", i, 2), ("tt", i)], writes=[("tt", i)])
            s.op("pool", f_tt(bufB[:, fo, :], mt[i][:], tt[i][:], ALU.add), reads=[("mt", i), ("tt", i)], writes=[("bufB", fo)])
        groups = []
        for br in range(3):
            groups.append(dict(nk=2, rhs=lambda kc: gdb[:, kc, :], rkeys=["gdb"],
                               wap=(lambda c0, cw, br=br: wgu[:, br * D + c0: br * D + c0 + cw])))
        groups.append(dict(nk=8, rhs=lambda kc: bufA[:, kc, :], rkeys=[("bufA", k) for k in range(0, 8)], wap=lambda c0, cw: wbh[:, c0:c0 + cw]))
        groups.append(dict(nk=8, rhs=lambda kc: bufA[:, 8 + kc, :], rkeys=[("bufA", k) for k in range(8, 16)], wap=lambda c0, cw: wbd[:, c0:c0 + cw]))
        groups.append(dict(nk=16, rhs=lambda kc: bufA[:, 16 + kc, :], rkeys=[("bufA", k) for k in range(16, 32)], wap=lambda c0, cw: wbr[:, c0:c0 + cw]))
        gemm(cx, groups, D, 256, epi1)
        def epi2(fo, outs, t0=t0):
            pt, pk = outs[0]
            i = cnt["x"] % 3; cnt["x"] += 1
            s.dma("sp", f_dma(cx.xs[i][:], xT[fo * 128:(fo + 1) * 128, t0:t0 + TS]), "xs%d" % i, writes=[("xs", i)])
            s.op("dve", f_tt(x1t[i][:], pt[:], cx.xs[i][:], ALU.add), reads=[pk, ("xs", i)], writes=[("x1t", i)])
            s.dma("sp", f_dma(outT[fo * 128:(fo + 1) * 128, t0:t0 + TS], x1t[i][:]), "x1o%d" % i, reads=[("x1t", i)], writes=[("xacc", fo)])
            j = fo % 2
            s.op("act", f_act(cx.sq[j][:], x1t[i][:], AF.Square), reads=[("x1t", i)], writes=[("sq", j)])
            s.group("pe", [f_mm(pss[:], cx.ones[:], cx.sq[j][:], fo == 0, fo == KC - 1)], reads=[("sq", j), "ones"], writes=[("ps", 7)])
            s.op("pool", f_ts(bufA[:, fo, :], x1t[i][:], fnw_sb[:, fo:fo + 1], None, ALU.mult), reads=[("x1t", i), "fnw"], writes=[("bufA", fo)])
        grp = dict(nk=32, rhs=lambda kc: bufB[:, kc, :], rkeys=[("bufB", k) for k in range(32)], wap=lambda c0, cw: wo[:, c0:c0 + cw])
        gemm(cx, [grp], D, 512, epi2)
        s.op("act", f_act(rstd[:], pss[:], AF.Sqrt, bias=EPS, scale=1.0 / D), reads=[("ps", 7)], writes=["rstd"])
        s.op("dve", f_recip(rstd[:], rstd[:]), reads=["rstd"], writes=["rstd"])
        for hh in range(2):
            def epi3(fl, outs):
                i = cnt["p3"] % 2; cnt["p3"] += 1
                s.op("dve", f_tt(tg[i][:], outs[0][0][:], rstd[:], ALU.mult), reads=[outs[0][1], "rstd"], writes=[("tg", i)])
                s.op("act", f_act(tsg[i][:], tg[i][:], AF.Silu), reads=[("tg", i)], writes=[("tsg", i)])
                s.op("dve", f_tt(tu[i][:], outs[1][0][:], rstd[:], ALU.mult), reads=[outs[1][1], "rstd"], writes=[("tu", i)])
                s.op("pool", f_tt(bufB[:, fl, :], tsg[i][:], tu[i][:], ALU.mult), reads=[("tsg", i), ("tu", i)], writes=[("bufB", fl)])
            h0 = hh * HH
            g1 = dict(nk=32, rhs=lambda kc: bufA[:, kc, :], rkeys=[("bufA", k) for k in range(32)], wap=lambda c0, cw, h0=h0: wg[:, h0 + c0:h0 + c0 + cw])
            g2 = dict(nk=32, rhs=lambda kc: bufA[:, kc, :], rkeys=[("bufA", k) for k in range(32)], wap=lambda c0, cw, h0=h0: wu[:, h0 + c0:h0 + c0 + cw])
            gemm(cx, [g1, g2], HH, 256, epi3)
            fin = last and hh == 1
            def epi4(fo, outs, t0=t0, fin=fin):
                pt, pk = outs[0]
                i = cnt["x"] % 3; cnt["x"] += 1
                s.dma("sp", f_dma(cx.xs[i][:], outT[fo * 128:(fo + 1) * 128, t0:t0 + TS]), "xs%d" % i, reads=[("xacc", fo)], writes=[("xs", i)])
                s.op("dve", f_tt(x1t[i][:], pt[:], cx.xs[i][:], ALU.add), reads=[pk, ("xs", i)], writes=[("x1t", i)])
                s.dma("sp", f_dma(outT[fo * 128:(fo + 1) * 128, t0:t0 + TS], x1t[i][:]), "x1o%d" % i, reads=[("x1t", i)], writes=[("xacc", fo)])
                if fin:
                    j = fo % 2
                    s.op("act", f_act(cx.sq[j][:], x1t[i][:], AF.Square), reads=[("x1t", i)], writes=[("sq", j)])
                    s.group("pe", [f_mm(pss[:], cx.ones[:], cx.sq[j][:], fo == 0, fo == KC - 1)], reads=[("sq", j), "ones"], writes=[("ps", 7)])
            g4 = dict(nk=NKH, rhs=lambda kc: bufB[:, kc, :], rkeys=[("bufB", k) for k in range(NKH)], wap=lambda c0, cw, h0=h0: wd[h0:h0 + HH, c0:c0 + cw])
            gemm(cx, [g4], D, 256, epi4)
        if last:
            s.op("act", f_act(rstd[:], pss[:], AF.Sqrt, bias=EPS, scale=1.0 / D), reads=[("ps", 7)], writes=["rstd"])
            s.op("dve", f_recip(rstd[:], rstd[:]), reads=["rstd"], writes=["rstd"])
            for fo in range(KC):
                i = cnt["x"] % 3; cnt["x"] += 1
                s.dma("sp", f_dma(cx.xs[i][:], outT[fo * 128:(fo + 1) * 128, t0:t0 + TS]), "xs%d" % i, reads=[("xacc", fo)], writes=[("xs", i)])
                s.op("dve", f_stt(x1t[i][:], cx.xs[i][:], lnw_sb[:, fo:fo + 1], rstd[:], ALU.mult, ALU.mult), reads=[("xs", i), "lnw", "rstd"], writes=[("x1t", i)])
                s.dma("sp", f_dma(outT[fo * 128:(fo + 1) * 128, t0:t0 + TS], x1t[i][:]), "x1o%d" % i, reads=[("x1t", i)], writes=[("xacc", fo)])

def build_phase_c(last, with_a=False):
    nc = bass.Bass("TRN2", target_bir_lowering=False)
    di = lambda name, shape, dt=F32: nc.dram_tensor(name, shape, dt, kind="ExternalInput").ap()
    yT = di("yT", [D, TOK], BF16); gdT = di("gdT", [GR, TOK], BF16); xT = di("xT", [D, TOK])
    wgu = di("wgu", [GR, 3 * D]); bg = di("bg", [128, 96])
    wbh = di("wbh", [1024, D]); wbd = di("wbd", [1024, D]); wbr = di("wbr", [2048, D]); wo = di("wo", [D, D])
    fnw = di("fnw", [128, KC]); wg = di("wg", [D, FFN]); wu = di("wu", [D, FFN]); wd = di("wd", [FFN, D])
    lnw = di("lnw", [128, KC])
    outT = nc.dram_tensor("outT", [D, TOK], F32, kind="ExternalOutput").ap()
    if with_a:
        w = di("w", [D, NIN]); anw = di("anw", [128, KC])
        out = nc.dram_tensor("projT", [NROW_F, TOK], F32, kind="ExternalOutput").ap()
        outb = nc.dram_tensor("projTb", [NROW_B, TOK], BF16, kind="ExternalOutput").ap()
    with contextlib.ExitStack() as es:
        s = Sched(nc, es)
        cx = Ctx(nc, es, s)
        with contextlib.ExitStack() as es2:
            cx.es = es2
            emit_phase_c(cx, yT, gdT, xT, wgu, bg, wbh, wbd, wbr, wo, fnw, wg, wu, wd, lnw, outT, last)
            sched_barrier(s); sched_flush(s)
        if with_a:
            with contextlib.ExitStack() as es2:
                cx.es = es2
                emit_phase_a(cx, outT, w, anw, out, outb)
                sched_barrier(s); sched_flush(s)
        s.finish(); sched_flush(s)
    return nc

def sigmoid(v): return 1 / (1 + np.exp(-v))
def ref_phase_c(y, gd, x, wgu, bgv, wbh, wbd, wbr, wo, fnwv, wg, wu, wd, lnwv, last):
    gates = sigmoid(gd @ wgu + bgv)
    gh, gdd, gr = gates[:, :D], gates[:, D:2 * D], gates[:, 2 * D:]
    merged = gh * (y[:, :1024] @ wbh) + gdd * (y[:, 1024:2048] @ wbd) + gr * (y[:, 2048:] @ wbr)
    x1 = x + merged @ wo
    h = x1 / np.sqrt((x1 * x1).mean(-1, keepdims=True) + EPS) * fnwv
    g = h @ wg
    x2 = x1 + ((g * sigmoid(g)) * (h @ wu)) @ wd
    if last:
        x2 = x2 / np.sqrt((x2 * x2).mean(-1, keepdims=True) + EPS) * lnwv
    return x2


S = 8192; SEG = 1024; NCH = SEG // 32
CL = 32

def f_scan(out, d0, d1, init, op0, op1):
    return lambda e: e.tensor_tensor_scan(out=out, data0=d0, data1=d1, initial=init, op0=op0, op1=op1)
def f_reduce(out, in_, axis, op):
    return lambda e: e.tensor_reduce(out=out, in_=in_, axis=axis, op=op)
def f_tr(out, in_, ident):
    return lambda e: e.transpose(out, in_, ident)
def f_acta(out, in_, func, accum):
    return lambda e: e.activation(out=out, in_=in_, func=func, accum_out=accum)

def bc_chunk(ap2d, n):
    return ap2d.unsqueeze(2).to_broadcast([ap2d.shape[0], ap2d.shape[1], n])
def bc_mid(ap2d, m):
    return ap2d.unsqueeze(1).to_broadcast([ap2d.shape[0], m, ap2d.shape[1]])

class LA:
    def __init__(self, cx, tag, nh, dv, ps_a, ps_o, ps_t, ps_u, mask, ident):
        self.cx = cx; self.tag = tag; self.nh = nh; self.dv = dv
        self.ps_a, self.ps_o, self.ps_t, self.ps_u = ps_a, ps_o, ps_t, ps_u
        self.mask = mask; self.ident = ident
        sb = cx.sb
        self.S = [sb(tag + "S%d" % j, [128, dv], F32) for j in range(nh)]
        self.Sb = [sb(tag + "Sb%d" % j, [128, dv], BF16) for j in range(nh)]
        self.A = [[sb(tag + "A%d_%d" % (j, i), [CL, CL], BF16) for i in range(2)] for j in range(nh)]
        self.Kt = [[sb(tag + "Kk%d_%d" % (j, i), [CL, 128], BF16) for i in range(2)] for j in range(nh)]
        s = cx.s
        for j in range(nh):
            s.op("dve", f_memset(self.S[j][:], 0.0), writes=[(tag, "S", j)])
            s.op("dve", f_memset(self.Sb[j][:], 0.0), writes=[(tag, "Sb", j)])
        self.n = 0
    def run_segment(self, Qt, Kt, Qh, Kh, dec, V, O, keys):
        s = self.cx.s; tag = self.tag; dv = self.dv
        for c in range(NCH):
            cs = slice(c * CL, (c + 1) * CL)
            for j in range(self.nh):
                i = self.n % 2
                k = keys[j]
                pa = self.ps_a[j][i]; po = self.ps_o[j][i]; ptt = self.ps_t[j][i]; pu = self.ps_u[j]
                A = self.A[j][i]; Kk = self.Kt[j][i]
                s.group("pe", [f_mm(pa[:], Kt[j][:, cs], Qt[j][:, cs], True, True)], reads=[k["Kt"], k["Qt"]], writes=[(tag, "pa", j, i)])
                s.op("dve", f_tt(A[:], pa[:], self.mask[:], ALU.mult), reads=[(tag, "pa", j, i), "mask"], writes=[(tag, "A", j, i)])
                s.group("pe", [f_tr(ptt[:], Kh[j][:, cs], self.ident[:])], reads=[k["Kh"], "ident"], writes=[(tag, "pt", j, i)])
                s.op("act", f_copy_act(Kk[:], ptt[:]), reads=[(tag, "pt", j, i)], writes=[(tag, "Kk", j, i)])
                s.group("pe", [f_mm(po[:], A[:], V[j][:, c, :], True, False),
                               f_mm(po[:], Qh[j][:, cs], self.Sb[j][:], False, True)],
                        reads=[(tag, "A", j, i), k["V"], k["Qh"], (tag, "Sb", j)], writes=[(tag, "po", j, i)])
                s.op("act", f_copy_act(O[j][:, c, :], po[:]), reads=[(tag, "po", j, i)], writes=[k["O"]])
                s.group("pe", [f_mm(pu[:], Kk[:], V[j][:, c, :], True, True)], reads=[(tag, "Kk", j, i), k["V"]], writes=[(tag, "pu", j)])
                s.op("dve", f_stt(self.S[j][:], self.S[j][:], dec[j][:, c:c + 1], pu[:], ALU.mult, ALU.add),
                     reads=[(tag, "S", j), k["dec"], (tag, "pu", j)], writes=[(tag, "S", j)])
                s.op("act", f_copy_act(self.Sb[j][:], self.S[j][:]), reads=[(tag, "S", j)], writes=[(tag, "Sb", j)])
            self.n += 1

def la_psum(cx, dv):
    pb, pbt = cx.pb, cx.pbt
    ps_a = [[pb[0][0:CL, (2 * j + i) * CL:(2 * j + i + 1) * CL] for i in range(2)] for j in range(2)]
    ps_o = [[pb[1 + j][0:CL, i * 256:i * 256 + dv] for i in range(2)] for j in range(2)]
    ps_t = [[pbt[0:CL, (2 * j + i) * 128:(2 * j + i + 1) * 128] for i in range(2)] for j in range(2)]
    ps_u = [pb[4 + j][:, 0:dv] for j in range(2)]
    return ps_a, ps_o, ps_t, ps_u

def alloc_b_psum(cx):
    cx.pb = [cx.es.enter_context(cx.nc.psum_tensor("pb%d" % i, [128, 512], F32)) for i in range(7)]
    cx.pbt = cx.es.enter_context(cx.nc.psum_tensor("pbt", [128, 1024], BF16))

def f_copy_act(out, in_):
    return lambda e: e.copy(out=out, in_=in_)

def la_backend(cx, s, tag, j, O, g_src, nw_bc, ydst, dv, bufs, okey):
    gt, ssq, rs, yb = bufs
    s.op("act", f_act(gt[:], O[:], AF.Square), reads=[okey], writes=[(tag, "gt")])
    s.op("dve", f_reduce(ssq[:], gt[:], AX.X, ALU.add), reads=[(tag, "gt")], writes=[(tag, "ssq")])
    s.op("act", f_act(rs[:], ssq[:], AF.Sqrt, bias=EPS, scale=1.0 / dv), reads=[(tag, "ssq")], writes=[(tag, "rs")])
    s.op("dve", f_recip(rs[:], rs[:]), reads=[(tag, "rs")], writes=[(tag, "rs")])
    s.dma("sp", f_dma(gt[:], g_src), tag + "g", reads=[(tag, "gt")], writes=[(tag, "gt")])
    s.op("act", f_act(gt[:], gt[:], AF.Silu), reads=[(tag, "gt")], writes=[(tag, "gt")])
    s.op("dve", f_tt(gt[:], gt[:], bc_mid(nw_bc, NCH), ALU.mult), reads=[(tag, "gt"), "nw" + tag], writes=[(tag, "gt")])
    s.op("dve", f_tt(O[:], O[:], bc_chunk(rs[:], dv), ALU.mult), reads=[okey, (tag, "rs")], writes=[okey])
    s.op("dve", f_tt(yb[:], gt[:], O[:], ALU.mult), reads=[(tag, "gt"), okey], writes=[(tag, "yb")])
    s.dma("sp", f_dma(ydst, yb[:]), tag + "y", reads=[(tag, "yb")])

def emit_hgrn(cx, layer, hq, hf, hi, hg, lbl, hnw, yh, consts):
    s = cx.s; sb = cx.sb; nc = cx.nc; es = cx.es
    mask, ident, rst = consts["mask"], consts["ident"], consts["rst"]
    ps_a, ps_o, ps_t, ps_u = la_psum(cx, 128)
    la = LA(cx, "H", 2, 128, ps_a, ps_o, ps_t, ps_u, mask, ident)
    lb = []; oml = []
    for j in range(2):
        lg = sb("h_lg%d" % j, [128, 4], F32); ssum = sb("h_ls%d" % j, [128, 1], F32)
        lbj = sb("h_lb%d" % j, [128, 1], F32); omlj = sb("h_oml%d" % j, [128, 1], F32)
        s.dma("sp", f_dma(lg[:], lbl[j]), "hlb%d" % j, writes=[("lg", j)])
        s.op("act", f_act(lg[:], lg[:], AF.Exp), reads=[("lg", j)], writes=[("lg", j)])
        s.op("dve", f_reduce(ssum[:], lg[:], AX.X, ALU.add), reads=[("lg", j)], writes=[("ls", j)])
        s.op("dve", f_recip(ssum[:], ssum[:]), reads=[("ls", j)], writes=[("ls", j)])
        if layer == 0:
            s.op("dve", f_memset(lbj[:], 0.0), writes=[("lb", j)])
        else:
            s.op("dve", f_reduce(lbj[:], lg[:, 1:layer + 1], AX.X, ALU.add), reads=[("lg", j)], writes=[("lb", j)])
            s.op("dve", f_tt(lbj[:], lbj[:], ssum[:], ALU.mult), reads=[("lb", j), ("ls", j)], writes=[("lb", j)])
        s.op("dve", f_ts(omlj[:], lbj[:], -1.0, 1.0, ALU.mult, ALU.add), reads=[("lb", j)], writes=[("oml", j)])
        lb.append(lbj); oml.append(omlj)
    nw_bc = sb("h_nw", [CL, 128], F32)
    s.dma("sp", f_dma(nw_bc[:], hnw.partition_broadcast(CL)), "hnw", writes=["nwH"])
    W = {}
    for nm in ("q", "f", "b", "t1", "t2"):
        W[nm, 0] = W[nm, 1] = sb("h_%s" % nm, [128, SEG], F32)
    for j in range(2):
        for nm in ("Qt", "Kt", "Qh", "Kh"):
            W[nm, j] = sb("h_%s%d" % (nm, j), [128, SEG], BF16)
        W["dec", j] = sb("h_dec%d" % j, [128, NCH], F32)
        W["V", j] = sb("h_V%d" % j, [CL, NCH, 128], BF16)
        W["O", j] = sb("h_O%d" % j, [CL, NCH, 128], F32)
    bufs = (sb("h_gt", [CL, NCH, 128], F32), sb("h_ssq", [CL, NCH], F32), sb("h_rs", [CL, NCH], F32), sb("h_yb", [CL, NCH, 128], BF16))
    sc = 128 ** -0.5
    for seg in range(S // SEG):
        t0 = seg * SEG
        keys = []
        for j in range(2):
            q, f, b, t1, t2 = (W[n, j] for n in ("q", "f", "b", "t1", "t2"))
            K = lambda n, j=j: ("H", n, j) if n in ("Qt", "Kt", "Qh", "Kh", "dec", "V", "O") else ("H", n)
            s.dma("sp", f_dma(q[:], hq[j][:, t0:t0 + SEG]), "hq%d" % j, writes=[K("q")])
            s.dma("sp", f_dma(f[:], hf[j][:, t0:t0 + SEG]), "hf%d" % j, writes=[K("f")])
            s.dma("pool", f_dma(W["V", j][:], hi[j][t0:t0 + SEG, :].rearrange("(c p) e -> p c e", p=CL)), "hV%d" % j, writes=[K("V")])
            s.op("act", f_act(q[:], q[:], AF.Silu), reads=[K("q")], writes=[K("q")])
            s.op("act", f_act(f[:], f[:], AF.Sigmoid), reads=[K("f")], writes=[K("f")])
            s.op("dve", f_ts(f[:], f[:], oml[j][:], lb[j][:], ALU.mult, ALU.add), reads=[K("f"), ("oml", j), ("lb", j)], writes=[K("f")])
            s.op("act", f_act(t1[:], f[:], AF.Ln), reads=[K("f")], writes=[K("t1")])
            s.op("dve", f_scan(b[:], rst[:], t1[:], 0.0, ALU.mult, ALU.add), reads=[K("t1"), "rst"], writes=[K("b")])
            s.op("pool", f_ts(f[:], f[:], -1.0, 1.0, ALU.mult, ALU.add), reads=[K("f")], writes=[K("f")])
            b3 = b[:].rearrange("p (c t) -> p c t", t=CL)
            bl = b3[:, :, CL - 1]; br = b3[:, :, CL // 2 - 1]
            s.op("act", f_act(W["dec", j][:], bl, AF.Exp), reads=[K("b")], writes=[K("dec")])
            s.op("act", f_act(t1[:], b[:], AF.Exp), reads=[K("b"), K("t1")], writes=[K("t1")])
            s.op("dve", f_stt(W["Qh", j][:], q[:], sc, t1[:], ALU.mult, ALU.mult), reads=[K("q"), K("t1")], writes=[K("Qh")])
            t13 = t1[:].rearrange("p (c t) -> p c t", t=CL); t23 = t2[:].rearrange("p (c t) -> p c t", t=CL)
            s.op("dve", f_tt(t23, bc_chunk(bl, CL), b3, ALU.subtract), reads=[K("b"), K("t2")], writes=[K("t2")])
            s.op("act", f_act(t2[:], t2[:], AF.Exp), reads=[K("t2")], writes=[K("t2")])
            s.op("pool", f_tt(W["Kh", j][:], f[:], t2[:], ALU.mult), reads=[K("f"), K("t2")], writes=[K("Kh")])
            s.op("dve", f_tt(t13, b3, bc_chunk(br, CL), ALU.subtract), reads=[K("b"), K("t1"), K("Qh")], writes=[K("t1")])
            s.op("dve", f_ts(t2[:], t1[:], -1.0, 40.0, ALU.mult, ALU.min), reads=[K("t1"), K("Kh")], writes=[K("t2")])
            s.op("pool", f_ts(t1[:], t1[:], 40.0, None, ALU.min), reads=[K("t1")], writes=[K("t1")])
            s.op("act", f_act(t1[:], t1[:], AF.Exp), reads=[K("t1")], writes=[K("t1")])
            s.op("act", f_act(t2[:], t2[:], AF.Exp), reads=[K("t2")], writes=[K("t2")])
            s.op("dve", f_stt(W["Qt", j][:], q[:], sc, t1[:], ALU.mult, ALU.mult), reads=[K("q"), K("t1")], writes=[K("Qt")])
            s.op("pool", f_tt(W["Kt", j][:], f[:], t2[:], ALU.mult), reads=[K("f"), K("t2")], writes=[K("Kt")])
            keys.append({n: K(n) for n in ("Qt", "Kt", "Qh", "Kh", "dec", "V", "O")})
        la.run_segment([W["Qt", j] for j in range(2)], [W["Kt", j] for j in range(2)], [W["Qh", j] for j in range(2)],
                       [W["Kh", j] for j in range(2)], [W["dec", j] for j in range(2)], [W["V", j] for j in range(2)],
                       [W["O", j] for j in range(2)], keys)
        for j in range(2):
            la_backend(cx, s, "H", j, W["O", j], hg[j][t0:t0 + SEG, :].rearrange("(c p) e -> p c e", p=CL), nw_bc[:],
                       yh[j][t0:t0 + SEG, :].rearrange("(c p) e -> p c e", p=CL), 128, bufs, ("H", "O", j))

def emit_ret(cx, rq, rk, cos2, sin2, rv, rg, rnw, rtab, yr):
    s = cx.s; sb = cx.sb; nc = cx.nc; es = cx.es
    mask, ident = cx.consts["mask"], cx.consts["ident"]
    ps_a, ps_o, ps_t, ps_u = la_psum(cx, 256)
    la = LA(cx, "R", 2, 256, ps_a, ps_o, ps_t, ps_u, mask, ident)
    nw_bc = sb("r_nw", [CL, 256], F32)
    s.dma("sp", f_dma(nw_bc[:], rnw.partition_broadcast(CL)), "rnw", writes=["nwR"])
    tab = [sb("r_tab%d" % j, [128, 5, CL], F32) for j in range(2)]
    dec = [sb("r_dec%d" % j, [128, NCH], F32) for j in range(2)]
    for j in range(2):
        s.dma("sp", f_dma(tab[j][:], rtab[j]), "rtab%d" % j, writes=[("rtab", j)])
        s.op("dve", f_copy(dec[j][:].rearrange("p (a c) -> p a c", c=CL), bc_mid(tab[j][:, 4, :], NCH // CL)), reads=[("rtab", j)], writes=[("R", "dec", j)])
    cs = sb("r_cos", [128, SEG], F32); sn = sb("r_sin", [128, SEG], F32)
    W = {}
    for nm in ("a", "as", "qr", "kr"):
        W[nm, 0] = W[nm, 1] = sb("r_%s" % nm, [128, SEG], F32)
    for j in range(2):
        for nm in ("Qt", "Kt", "Qh", "Kh"):
            W[nm, j] = sb("r_%s%d" % (nm, j), [128, SEG], BF16)
        W["V", j] = sb("r_V%d" % j, [CL, NCH, 256], BF16)
        W["O", j] = sb("r_O%d" % j, [CL, NCH, 256], F32)
    bufs = (sb("r_gt", [CL, NCH, 256], F32), sb("r_ssq", [CL, NCH], F32), sb("r_rs", [CL, NCH], F32), sb("r_yb", [CL, NCH, 256], BF16))
    for seg in range(S // SEG):
        t0 = seg * SEG
        s.dma("sp", f_dma(cs[:], cos2[:, t0:t0 + SEG]), "rcos", writes=["rcos"])
        s.dma("sp", f_dma(sn[:], sin2[:, t0:t0 + SEG]), "rsin", writes=["rsin"])
        keys = []
        for j in range(2):
            K = lambda n, j=j: ("R", n, j) if n in ("Qt", "Kt", "Qh", "Kh", "dec", "V", "O") else ("R", n)
            a, as_, qr, kr = (W[n, j] for n in ("a", "as", "qr", "kr"))
            for (src, dst, dk) in ((rq, qr, "qr"), (rk, kr, "kr")):
                s.dma("sp", f_dma(a[:], src[j][:, t0:t0 + SEG]), "ra%d" % j, writes=[K("a")])
                s.dma("sp", f_dma(as_[0:64, :], src[j][64:128, t0:t0 + SEG]), "ras%d" % j, writes=[K("as")])
                s.dma("sp", f_dma(as_[64:128, :], src[j][0:64, t0:t0 + SEG]), "rasb%d" % j, writes=[K("as")])
                s.op("dve", f_tt(a[:], a[:], cs[:], ALU.mult), reads=[K("a"), "rcos"], writes=[K("a")])
                s.op("pool", f_tt(as_[:], as_[:], sn[:], ALU.mult), reads=[K("as"), "rsin"], writes=[K("as")])
                s.op("dve", f_tt(dst[:], a[:], as_[:], ALU.add), reads=[K("a"), K("as")], writes=[K(dk)])
            s.dma("pool", f_dma(W["V", j][:], rv[j][t0:t0 + SEG, :].rearrange("(c p) e -> p c e", p=CL)), "rV%d" % j, writes=[K("V")])
            for ti, (nm, srcb, eng) in enumerate((("Qt", qr, "dve"), ("Kt", kr, "pool"), ("Qh", qr, "dve"), ("Kh", kr, "pool"))):
                s.op(eng, f_tt(W[nm, j][:].rearrange("p (c t) -> p c t", t=CL), srcb[:].rearrange("p (c t) -> p c t", t=CL),
                               bc_mid(tab[j][:, ti, :], NCH), ALU.mult), reads=[K("qr" if srcb is qr else "kr"), ("rtab", j)], writes=[K(nm)])
            keys.append({n: K(n) for n in ("Qt", "Kt", "Qh", "Kh", "dec", "V", "O")})
        la.run_segment([W["Qt", j] for j in range(2)], [W["Kt", j] for j in range(2)], [W["Qh", j] for j in range(2)],
                       [W["Kh", j] for j in range(2)], dec, [W["V", j] for j in range(2)], [W["O", j] for j in range(2)], keys)
        for j in range(2):
            la_backend(cx, s, "R", j, W["O", j], rg[j][t0:t0 + SEG, :].rearrange("(c p) e -> p c e", p=CL), nw_bc[:],
                       yr[j][t0:t0 + SEG, :].rearrange("(c p) e -> p c e", p=CL), 256, bufs, ("R", "O", j))

def load_consts(cx, mask_d, ident_d, rst_d):
    s = cx.s
    mask = cx.sb("c_mask", [CL, CL], F32); ident = cx.sb("c_ident", [128, 128], BF16); rst = cx.sb("c_rst", [128, SEG], F32)
    s.dma("sp", f_dma(mask[:], mask_d), "cmask", writes=["mask"])
    s.dma("pool", f_dma(ident[:], ident_d), "cident", writes=["ident"])
    s.dma("sp", f_dma(rst[:], rst_d), "crst", writes=["rst"])
    cx.consts = {"mask": mask, "ident": ident, "rst": rst}
    return cx.consts

def host_consts():
    mask = np.triu(np.ones((CL, CL), np.float32))
    ident = np.eye(128, dtype=np.float32)
    rst = np.ones((128, SEG), np.float32); rst[:, ::CL] = 0.0
    return mask, ident, rst

def host_ret_tables(head):
    lg = np.log(1.0 - 2.0 ** (-5.0 - head))
    t = np.arange(CL, dtype=np.float64)
    sc = 128 ** -0.5
    tab = np.stack([np.exp(lg * (t - 15)), sc * np.exp(lg * (15 - t)), np.exp(lg * (t + 1)), sc * np.exp(lg * (31 - t)),
                    np.full(CL, np.exp(lg * CL))], 0)
    return np.broadcast_to(tab[None], (128, 5, CL)).astype(np.float32).copy()

def host_rope_tables():
    half = 64
    inv = (1.0 / (10000.0 ** (np.arange(half, dtype=np.float32) / half))).astype(np.float32)
    ang = (np.arange(S, dtype=np.float32)[:, None] * inv[None, :]).astype(np.float32)
    cos, sin = np.cos(ang).astype(np.float32).T, np.sin(ang).astype(np.float32).T
    return np.concatenate([cos, cos], 0).copy(), np.concatenate([-sin, sin], 0).copy()

QS = 256; NQS = S // QS; NKB = S // 128

def host_diff_consts():
    k = np.arange(128)[:, None]; q = np.arange(128)[None, :]
    return np.concatenate([(q - k), (128 + q - k)], 1).astype(np.float32)

def t5_steps():
    n = np.arange(0, 256)
    nf = np.maximum(n, 1).astype(np.float32)
    large = 16 + (np.log(nf / np.float32(16)) / np.float32(np.log(128 / 16)) * np.float32(16)).astype(np.int32)
    large = np.minimum(large, 31)
    b = np.where(n < 16, n, large)
    steps = [(int(i), int(b[i]), int(b[i - 1])) for i in range(1, 256) if b[i] != b[i - 1]]
    return int(b[0]), steps

def emit_diff(cx, layer, dq, dk, dv_, relb, dlam, dnw, dmat, yd, stage=1):
    import math
    s = cx.s; sb = cx.sb
    lam_init = 0.8 - 0.6 * math.exp(-0.3 * layer)
    pb = cx.pb
    dl = sb("d_dl", [128, 256], F32); pr = sb("d_pr", [128, 128], F32); s12 = sb("d_s12", [128, 2], F32); nlam = sb("d_nlam", [128, 1], F32)
    s.dma("sp", f_dma(dl[:], dlam.partition_broadcast(128)), "ddl", writes=["dl"])
    dl3 = dl[:].rearrange("p (a d) -> p a d", d=64)
    s.op("dve", f_tt(pr[:].rearrange("p (a d) -> p a d", d=64), dl3[:, 0::2, :], dl3[:, 1::2, :], ALU.mult), reads=["dl"], writes=["pr"])
    s.op("dve", f_reduce(s12[:], pr[:].rearrange("p (a d) -> p a d", d=64), AX.X, ALU.add), reads=["pr"], writes=["s12"])
    s.op("act", f_act(s12[:], s12[:], AF.Exp), reads=["s12"], writes=["s12"])
    s.op("dve", f_tt(nlam[:], s12[:, 1:2], s12[:, 0:1], ALU.subtract), reads=["s12"], writes=["nlam"])
    s.op("dve", f_ts(nlam[:], nlam[:], -lam_init, None, ALU.add), reads=["nlam"], writes=["nlam"])
    nw = sb("d_nw", [128, 128], F32)
    s.dma("sp", f_dma(nw[:], dnw.partition_broadcast(128)), "dnw", writes=["dnw"])
    s.op("dve", f_ts(nw[:], nw[:], 1.0 - lam_init, None, ALU.mult), reads=["dnw"], writes=["dnw"])
    dm = sb("d_dm", [128, 256], F32)
    s.dma("sp", f_dma(dm[:], dmat), "ddm", writes=["dm"])
    b0, steps = t5_steps()
    E = []
    for j in range(2):
        rb = sb("d_rb%d" % j, [128, 32], F32); acc = sb("d_acc%d" % j, [128, 256], F32); tmp = sb("d_tmp%d" % j, [128, 256], F32)
        dlt = sb("d_dlt%d" % j, [128, 1], F32); Ej = sb("d_E%d" % j, [128, 256], BF16)
        s.dma("sp", f_dma(rb[:], relb[j].partition_broadcast(128)), "drb%d" % j, writes=[("rb", j)])
        s.op("dve", f_tt(dlt[:], rb[:, b0:b0 + 1], rb[:, 31:32], ALU.subtract), reads=[("rb", j)], writes=[("dlt", j)])
        s.op("dve", f_ts(acc[:], dm[:], 0.0, dlt[:], ALU.mult, ALU.add), reads=["dm", ("dlt", j)], writes=[("acc", j)])
        for (n, bn, bo) in steps:
            s.op("dve", f_tt(dlt[:], rb[:, bn:bn + 1], rb[:, bo:bo + 1], ALU.subtract), reads=[("rb", j), ("dlt", j)], writes=[("dlt", j)])
            s.op("dve", f_ts(tmp[:], dm[:], float(n), dlt[:], ALU.is_ge, ALU.mult), reads=["dm", ("dlt", j)], writes=[("tmp", j)])
            s.op("dve", f_tt(acc[:], acc[:], tmp[:], ALU.add), reads=[("acc", j), ("tmp", j)], writes=[("acc", j)])
        s.op("act", f_act(acc[:], acc[:], AF.Exp), reads=[("acc", j)], writes=[("acc", j)])
        s.op("dve", f_ts(tmp[:], dm[:], 0.0, None, ALU.is_ge), reads=["dm"], writes=[("tmp", j)])
        s.op("dve", f_tt(Ej[:], acc[:], tmp[:], ALU.mult), reads=[("acc", j), ("tmp", j)], writes=[("E", j)])
        E.append(Ej)
    if stage == 0:
        dbg = sb("d_dbg", [128, 256], F32)
        s.op("dve", f_copy(dbg[:], E[0][:]), reads=[("E", 0)], writes=["dbg"])
        s.dma("sp", f_dma(yd[0][0:128, :], dbg[:, 0:128]), "dbg0", reads=["dbg"])
        s.dma("sp", f_dma(yd[0][128:256, :], dbg[:, 128:256]), "dbg1", reads=["dbg"])
        s.op("dve", f_ts(dbg[:, 0:128], dm[:, 0:128], 0.0, nlam[:], ALU.mult, ALU.add), reads=["dbg", "nlam", "dm"], writes=["dbg2"]); s.dma("sp", f_dma(yd[1][0:128, :], dbg[:, 0:128]), "dbg2", reads=["dbg2"])
        s.dma("sp", f_dma(yd[1][128:256, :], nw[:]), "dbg3", reads=["dnw"])
        return
    QTa = sb("d_QTa", [128, S], BF16); QTb = sb("d_QTb", [128, S], BF16); KT = sb("d_KT", [128, S], BF16); VX = sb("d_VX", [128, NKB, 132], BF16)
    P = [sb("d_P%d" % i, [128, 512], BF16) for i in range(3)]
    ut = [sb("d_u%d" % i, [128, 128], F32) for i in range(2)]; tt_ = [sb("d_t%d" % i, [128, 128], F32) for i in range(2)]
    yt = [sb("d_y%d" % i, [128, 128], BF16) for i in range(2)]; junk = sb("d_junk", [128, 128], F32)
    sm = [sb("d_sm%d" % i, [128, 4], F32) for i in range(2)]
    ps_s = [pb[0], pb[1], pb[2]]
    ps_o = [[pb[3][:, 0:129], pb[4][:, 0:129]], [pb[5][:, 0:129], pb[6][:, 0:129]]]
    s.op("dve", f_memset(VX[:, :, 128:129], 1.0), writes=["VXone"])
    s.op("dve", f_memset(QTa[64:128, :], 0.0), writes=["QTz"])
    s.op("pool", f_memset(QTb[0:64, :], 0.0), writes=["QTz2"])
    it = 0; fin = 0
    for j in range(2):
        for q4 in range(4):
            sl = slice(q4 * (S // 4), (q4 + 1) * (S // 4))
            s.dma("pool", f_dma(QTa[0:64, sl], dq[j][0:64, sl]), "dQT", writes=["QT"])
            s.dma("pool", f_dma(QTb[64:128, sl], dq[j][64:128, sl]), "dQT2", writes=["QT"])
            s.dma("pool", f_dma(KT[:, sl], dk[j][:, sl]), "dKT", writes=["KT"])
        s.dma("pool", f_dma(VX[:, :, 0:128], dv_[j].rearrange("(kb p) e -> p kb e", p=128)), "dVX", writes=["VX"])
        for qs in range(NQS):
            qb0 = 2 * qs; qb1 = qb0 + 1
            qsl = slice(qs * QS, (qs + 1) * QS)
            for kb in range(qb1 + 1):
                ksl = slice(kb * 128, (kb + 1) * 128)
                i = it % 3; it += 1
                pss = ps_s[i]; Pt = P[i]
                s.group("pe", [f_mm(pss[:, 0:256], KT[:, ksl], QTa[:, qsl], True, True),
                               f_mm(pss[:, 256:512], KT[:, ksl], QTb[:, qsl], True, True)],
                        reads=["QT", "KT", "QTz", "QTz2"], writes=[("pss", i)])
                s.op("act", f_act(Pt[:], pss[:], AF.Exp, scale=0.125), reads=[("pss", i)], writes=[("P", i)])
                P3 = Pt[:].rearrange("p (m q) -> p m q", m=2)
                def mul(qoff, eoff):
                    s.op("dve", f_tt(P3[:, :, qoff:qoff + 128], P3[:, :, qoff:qoff + 128], bc_mid(E[j][:, eoff:eoff + 128], 2), ALU.mult),
                         reads=[("P", i), ("E", j)], writes=[("P", i)])
                jqs = [0, 1]
                if kb == qb0 - 1: mul(0, 128)
                if kb == qb0: mul(0, 0); mul(128, 128)
                if kb == qb1: mul(128, 0); jqs = [1]
                fns = []; wr = []
                for jq in jqs:
                    last = qb0 if jq == 0 else qb1
                    for m in range(2):
                        fns.append(f_mm(ps_o[jq][m], Pt[:, m * 256 + jq * 128:m * 256 + jq * 128 + 128], VX[:, kb, 0:129], kb == 0, kb == last))
                        wr.append(("pso", jq, m))
                s.group("pe", fns, reads=[("P", i), "VX", "VXone"], writes=wr)
                for jq in jqs:
                    last = qb0 if jq == 0 else qb1
                    if kb != last: continue
                    f = fin % 2; fin += 1
                    O1, O2 = ps_o[jq][0], ps_o[jq][1]
                    k1, k2 = ("pso", jq, 0), ("pso", jq, 1)
                    s.op("dve", f_recip(sm[f][:, 0:1], O1[:, 128:129]), reads=[k1], writes=[("sm", f)])
                    s.op("dve", f_recip(sm[f][:, 1:2], O2[:, 128:129]), reads=[k2, ("sm", f)], writes=[("sm", f)])
                    s.op("dve", f_tt(sm[f][:, 1:2], sm[f][:, 1:2], nlam[:], ALU.mult), reads=[("sm", f), "nlam"], writes=[("sm", f)])
                    s.op("act", f_act(tt_[f][:], O1[:, 0:128], AF.Copy, scale=sm[f][:, 0:1]), reads=[k1, ("sm", f)], writes=[("tt", f)])
                    s.op("dve", f_stt(ut[f][:], O2[:, 0:128], sm[f][:, 1:2], tt_[f][:], ALU.mult, ALU.add), reads=[k2, ("sm", f), ("tt", f)], writes=[("ut", f)])
                    s.op("act", f_act(junk[:], ut[f][:], AF.Square), reads=[("ut", f)], writes=["junk"])
                    s.op("dve", f_reduce(sm[f][:, 2:3], junk[:], AX.X, ALU.add), reads=["junk", ("sm", f)], writes=[("sm", f)])
                    s.op("act", f_act(sm[f][:, 3:4], sm[f][:, 2:3], AF.Sqrt, bias=EPS, scale=1.0 / 128), reads=[("sm", f)], writes=[("sm", f)])
                    s.op("dve", f_recip(sm[f][:, 3:4], sm[f][:, 3:4]), reads=[("sm", f)], writes=[("sm", f)])
                    s.op("dve", f_stt(yt[f][:], ut[f][:], sm[f][:, 3:4], nw[:], ALU.mult, ALU.mult), reads=[("ut", f), ("sm", f), "dnw"], writes=[("yt", f)])
                    r0 = (qb0 + jq) * 128
                    s.dma("sp", f_dma(yd[j][r0:r0 + 128, :], yt[f][:]), "dy%d" % f, reads=[("yt", f)])


DEPTH = 4
_PROGS = {}

def _prog_a():
    if "A" not in _PROGS:
        _PROGS["A"] = build_phase_a()
    return _PROGS["A"]

def _prog_c(last, with_a):
    k = ("C", bool(last), bool(with_a))
    if k not in _PROGS:
        _PROGS[k] = build_phase_c(bool(last), bool(with_a))
    return _PROGS[k]

def _prog_b(layer):
    k = ("B", layer)
    if k in _PROGS:
        return _PROGS[k]
    nc = bass.Bass("TRN2", target_bir_lowering=False)
    di = lambda name, shape, dt=F32: nc.dram_tensor(name, shape, dt, kind="ExternalInput").ap()
    do = lambda name, shape: nc.dram_tensor(name, shape, BF16, kind="ExternalOutput").ap()
    mask_d = di("mask", [CL, CL]); ident_d = di("ident", [128, 128]); rst_d = di("rst", [128, SEG])
    hq = di("hq", [2, 128, S]); hf = di("hf", [2, 128, S]); hi = di("hi", [2, S, 128], BF16); hg = di("hg", [2, S, 128])
    lbl = di("lbl", [2, 128, 4]); hnw = di("hnw", [128]); yh = do("yh", [2, S, 128])
    rq = di("rq", [2, 128, S]); rk = di("rk", [2, 128, S])
    cos2 = di("cos2", [128, S]); sin2 = di("sin2", [128, S]); rv = di("rv", [2, S, 256], BF16); rg = di("rg", [2, S, 256])
    rnw = di("rnw", [256]); rtab = di("rtab", [2, 128, 5, CL]); yr = do("yr", [2, S, 256])
    dq = di("dq", [2, 128, S], BF16); dk = di("dk", [2, 128, S], BF16); dv_ = di("dv", [2, S, 128], BF16); relb = di("relb", [2, 32])
    dlam = di("dlam", [256]); dnw = di("dnw", [128]); dmat = di("dmat", [128, 256]); yd = do("yd", [2, S, 128])
    with contextlib.ExitStack() as es0:
        s = Sched(nc, es0)
        with contextlib.ExitStack() as es:
            cx = Ctx(nc, es, s, gemm=False); alloc_b_psum(cx)
            load_consts(cx, mask_d, ident_d, rst_d)
            with contextlib.ExitStack() as es2:
                cx.es = es2
                emit_hgrn(cx, layer, hq, hf, hi, hg, lbl, hnw, yh, cx.consts)
                sched_barrier(s); sched_flush(s)
            with contextlib.ExitStack() as es2:
                cx.es = es2
                emit_ret(cx, rq, rk, cos2, sin2, rv, rg, rnw, rtab, yr)
                sched_barrier(s); sched_flush(s)
            with contextlib.ExitStack() as es2:
                cx.es = es2
                emit_diff(cx, layer, dq, dk, dv_, relb, dlam, dnw, dmat, yd)
                sched_barrier(s); sched_flush(s)
            s.finish(); sched_flush(s)
    _PROGS[k] = nc
    return nc

_OFF = {"hq": 0, "hf": 1024, "hi": 2048, "hg": 3072, "dq": 4096, "dk": 5120, "dv": 6144,
        "rq": 7168, "rk": 8192, "rv": 9216, "rg": 11264, "gd": 13312}

def _lay(v):
    return np.ascontiguousarray(np.asarray(v, np.float32).reshape(-1, 128).T)

def _rows(PTf, PTb, name, r0, n):
    a = _OFF[name] + r0
    isb, o = _ROWMAP[a // 128]
    o += a % 128
    return (PTb if isb else PTf)[o:o + n, :]

def kernel(x, attn_norm_w, w_in, lb_logits, hgrn_norm_w, rel_bias, diff_lambda, diff_norm_w, ret_norm_w, w_gate_up,
           b_gate, w_br_hgrn, w_br_diff, w_br_ret, w_o, ffn_norm_w, w_ffn_gate, w_ffn_up, w_ffn_down, final_norm_w):
    f32 = lambda a: np.asarray(a, np.float32)
    x = f32(x)
    NCORE = 8; cores = list(range(NCORE))
    xT = [np.ascontiguousarray(x[c // 4, (c % 4) * TOK:(c % 4 + 1) * TOK, :].T) for c in cores]
    mask, ident, rst = host_consts()
    cos2, sin2 = host_rope_tables()
    dmat = host_diff_consts()
    rtabs = [host_ret_tables(h) for h in range(8)]
    lbl_all = f32(lb_logits)
    res = run_bass_kernel_spmd(_prog_a(), [{"xT": xT[c], "w": f32(w_in[0]), "anw": _lay(attn_norm_w[0])} for c in cores], core_ids=cores)
    pf = [res.results[c]["projT"] for c in cores]; pb_ = [res.results[c]["projTb"] for c in cores]
    del res
    for l in range(DEPTH):
        PTf = [np.concatenate(pf[b * 4:(b + 1) * 4], axis=1) for b in range(2)]
        PTb = [np.concatenate(pb_[b * 4:(b + 1) * 4], axis=1) for b in range(2)]
        gdT = [np.ascontiguousarray(_rows(pf[c], pb_[c], "gd", 0, GR)) for c in cores]
        del pf, pb_
        in_b = []
        for c in cores:
            b, hp = c // 4, c % 4
            fm = lambda name, d: np.ascontiguousarray(_rows(PTf[b], PTb[b], name, 2 * hp * d, 2 * d).reshape(2, d, S))
            tm = lambda name, d: np.ascontiguousarray(_rows(PTf[b], PTb[b], name, 2 * hp * d, 2 * d).reshape(2, d, S).transpose(0, 2, 1))
            in_b.append({
                "mask": mask, "ident": ident, "rst": rst,
                "hq": fm("hq", 128), "hf": fm("hf", 128), "hi": tm("hi", 128), "hg": tm("hg", 128),
                "lbl": np.ascontiguousarray(lbl_all[:, 2 * hp * 128:2 * (hp + 1) * 128].T.reshape(2, 128, 4)),
                "hnw": f32(hgrn_norm_w[l]),
                "rq": fm("rq", 128), "rk": fm("rk", 128), "cos2": cos2, "sin2": sin2,
                "rv": tm("rv", 256), "rg": tm("rg", 256), "rnw": f32(ret_norm_w[l]),
                "rtab": np.stack([rtabs[2 * hp], rtabs[2 * hp + 1]], 0),
                "dq": fm("dq", 128), "dk": fm("dk", 128), "dv": tm("dv", 128),
                "relb": np.ascontiguousarray(f32(rel_bias)[:, 2 * hp:2 * hp + 2].T),
                "dlam": np.ascontiguousarray(f32(diff_lambda[l]).reshape(256)), "dnw": f32(diff_norm_w[l]), "dmat": dmat,
            })
        del PTf, PTb
        res = run_bass_kernel_spmd(_prog_b(l), in_b, core_ids=cores)
        del in_b
        ydt = res.results[0]["yh"].dtype
        Y = [np.empty((S, D), ydt) for _ in range(2)]
        for c in cores:
            b, hp = c // 4, c % 4
            r = res.results[c]
            for j in range(2):
                h = 2 * hp + j
                Y[b][:, h * 128:(h + 1) * 128] = r["yh"][j]
                Y[b][:, 1024 + h * 128:1024 + (h + 1) * 128] = r["yd"][j]
                Y[b][:, 2048 + h * 256:2048 + (h + 1) * 256] = r["yr"][j]
        del res
        yT = [np.ascontiguousarray(Y[c // 4][(c % 4) * TOK:(c % 4 + 1) * TOK, :].T) for c in cores]
        del Y
        last = (l == DEPTH - 1)
        wts = {"wgu": f32(w_gate_up[l]), "bg": _lay(b_gate[l]), "wbh": f32(w_br_hgrn[l]), "wbd": f32(w_br_diff[l]),
               "wbr": f32(w_br_ret[l]), "wo": f32(w_o[l]), "fnw": _lay(ffn_norm_w[l]), "wg": f32(w_ffn_gate[l]),
               "wu": f32(w_ffn_up[l]), "wd": f32(w_ffn_down[l]), "lnw": _lay(final_norm_w)}
        if not last:
            wts["w"] = f32(w_in[l + 1]); wts["anw"] = _lay(attn_norm_w[l + 1])
        res = run_bass_kernel_spmd(_prog_c(last, not last), [dict(wts, yT=yT[c], gdT=gdT[c], xT=xT[c]) for c in cores], core_ids=cores)
        xT = [res.results[c]["outT"] for c in cores]
        if not last:
            pf = [res.results[c]["projT"] for c in cores]; pb_ = [res.results[c]["projTb"] for c in cores]
        del res, yT, gdT, wts
    out = np.empty((2, S, D), np.float32)
    for c in cores:
        out[c // 4, (c % 4) * TOK:(c % 4 + 1) * TOK, :] = xT[c].T
    return out
```

```python
import contextlib, numpy as np
import concourse.bass as bass
import concourse.mybir as mybir
from concourse.bass_utils import run_bass_kernel_spmd
F32 = mybir.dt.float32; BF16 = mybir.dt.bfloat16
ALU = mybir.AluOpType; AF = mybir.ActivationFunctionType; AX = mybir.AxisListType

class Sched:
    ENGS = ("pe", "act", "dve", "pool", "sp")
    def __init__(self, nc, es, n_dma_sems=40):
        self.nc = nc; self.es = es
        self.streams = {e: [] for e in self.ENGS}
        self.esem = {e: es.enter_context(nc.semaphore("es_" + e)) for e in ("pe", "act", "dve", "pool")}
        self.ecnt = {e: 0 for e in self.esem}
        self.waited = {e: {} for e in self.ENGS}
        self.lastw = {}; self.reads = {}
        self.dsem = {}; self.dcnt = {}
        self.n_dma = 0
    def _dma_sem(self, key):
        if key not in self.dsem:
            self.dsem[key] = self.es.enter_context(self.nc.semaphore("ds_%d" % len(self.dsem)))
            self.dcnt[key] = 0
        return self.dsem[key]
    def _deps(self, eng, reads, writes):
        deps = []
        for b in list(reads) + list(writes):
            if b in self.lastw: deps.append(self.lastw[b])
        for b in writes:
            deps.extend(self.reads.get(b, ()))
        need = {}
        for (sem, val) in deps:
            k = id(sem)
            if self.waited[eng].get(k, 0) >= val: continue
            if k not in need or need[k][1] < val: need[k] = (sem, val)
        for k, (sem, val) in need.items():
            self.waited[eng][k] = val
            self.streams[eng].append(("wait", sem, val))
    def _commit(self, tick, reads, writes):
        for b in writes:
            self.lastw[b] = tick; self.reads[b] = []
        for b in reads:
            self.reads.setdefault(b, []).append(tick)
    def op(self, eng, fn, reads=(), writes=()):
        self._deps(eng, reads, writes)
        self.ecnt[eng] += 1
        tick = (self.esem[eng], self.ecnt[eng])
        self.streams[eng].append(("op", fn, self.esem[eng], 1))
        self.waited[eng][id(self.esem[eng])] = max(self.waited[eng].get(id(self.esem[eng]), 0), 0)
        self._commit(tick, reads, writes)
    def group(self, eng, fns, reads=(), writes=()):
        self._deps(eng, reads, writes)
        for f in fns[:-1]:
            self.streams[eng].append(("op", f, None, 0))
        self.ecnt[eng] += 1
        tick = (self.esem[eng], self.ecnt[eng])
        self.streams[eng].append(("op", fns[-1], self.esem[eng], 1))
        self._commit(tick, reads, writes)
    def dma(self, eng, fn, semkey, reads=(), writes=()):
        sem = self._dma_sem(semkey)
        self._deps(eng, reads, writes)
        self.dcnt[semkey] += 16
        tick = (sem, self.dcnt[semkey])
        self.streams[eng].append(("op", fn, sem, 16))
        self._commit(tick, reads, writes)
    def finish(self):
        for key, sem in self.dsem.items():
            if self.dcnt[key]: self.streams["sp"].append(("wait", sem, self.dcnt[key]))
        for e, sem in self.esem.items():
            if self.ecnt[e]: self.streams["sp"].append(("wait", sem, self.ecnt[e]))
    def replay(self):
        nc = self.nc
        def run(engobj, items):
            for it in items:
                if it[0] == "wait":
                    engobj.wait_ge(it[1], it[2])
                else:
                    ins = it[1](engobj)
                    if it[2] is not None:
                        ins.then_inc(it[2], it[3])
        with nc.Block() as block:
            @block.sync
            def _(e): run(e, self.streams["sp"])
            @block.tensor
            def _(e): run(e, self.streams["pe"])
            @block.scalar
            def _(e): run(e, self.streams["act"])
            @block.vector
            def _(e): run(e, self.streams["dve"])
            @block.gpsimd
            def _(e): run(e, self.streams["pool"])
    def stats(self):
        return {e: len(v) for e, v in self.streams.items()}

def sched_barrier(s):
    for eng in s.ENGS:
        for e2, sem in s.esem.items():
            v = s.ecnt[e2]
            if v and s.waited[eng].get(id(sem), 0) < v:
                s.streams[eng].append(("wait", sem, v)); s.waited[eng][id(sem)] = v
        for key, sem in s.dsem.items():
            v = s.dcnt[key]
            if v and s.waited[eng].get(id(sem), 0) < v:
                s.streams[eng].append(("wait", sem, v)); s.waited[eng][id(sem)] = v
    s.lastw = {}; s.reads = {}

def sched_flush(s):
    s.replay()
    for e in s.streams: s.streams[e] = []


D = 4096; KC = 32; TOK = 2048; TS = 512; NIN = 13568; FFN = 11008; GR = 256
EPS = 1e-6
WMAX = 16384

def f_mm(out, lhsT, rhs, start, stop):
    return lambda e: e.matmul(out, lhsT=lhsT, rhs=rhs, start=start, stop=stop)
def f_dma(out, in_):
    return lambda e: e.dma_start(out=out, in_=in_)
def f_act(out, in_, func, bias=None, scale=None):
    kw = {}
    if bias is not None: kw["bias"] = bias
    if scale is not None: kw["scale"] = scale
    return lambda e: e.activation(out=out, in_=in_, func=func, **kw)
def f_tt(out, in0, in1, op):
    return lambda e: e.tensor_tensor(out=out, in0=in0, in1=in1, op=op)
def f_ts(out, in0, s1, s2, op0, op1=None):
    if op1 is None:
        return lambda e: e.tensor_scalar(out=out, in0=in0, scalar1=s1, scalar2=None, op0=op0)
    return lambda e: e.tensor_scalar(out=out, in0=in0, scalar1=s1, scalar2=s2, op0=op0, op1=op1)
def f_stt(out, in0, scalar, in1, op0, op1):
    return lambda e: e.scalar_tensor_tensor(out=out, in0=in0, scalar=scalar, in1=in1, op0=op0, op1=op1)
def f_copy(out, in_):
    return lambda e: e.tensor_copy(out=out, in_=in_)
def f_recip(out, in_):
    return lambda e: e.reciprocal(out=out, in_=in_)
def f_memset(ap, v):
    return lambda e: e.memset(ap, v)

class Ctx:
    def __init__(self, nc, es, s, nps=7, gemm=True):
        self.nc = nc; self.es = es; self.s = s
        self.nps = nps; self.rr = 0
        if gemm:
            self.ps = [es.enter_context(nc.psum_tensor("ps%d" % i, [128, 512], F32)) for i in range(8)]
            self.wb = [es.enter_context(nc.sbuf_tensor("wb%d" % i, [128, WMAX], BF16)) for i in range(2)]
        self.wcount = 0
        self.ones = es.enter_context(nc.sbuf_tensor("ones", [128, 128], BF16))
        s.op("dve", f_memset(self.ones[:], 1.0), writes=["ones"])
        self._n = 0
    def sb(self, name, shape, dt):
        return self.es.enter_context(self.nc.sbuf_tensor(name, shape, dt))
    def next_ps(self):
        i = self.rr % self.nps; self.rr += 1
        return self.ps[i], ("ps", i)

def gemm(cx, groups, ncols, CB, epilogue, fo_base=0):
    s = cx.s
    ncb = (ncols + CB - 1) // CB
    secoff = []; off = 0
    for g in groups:
        secoff.append(off); off += g["nk"] * CB
    assert off <= WMAX, off
    def issue_w(cb):
        c0 = cb * CB; cw = min(CB, ncols - c0)
        slot = cx.wcount % 2; cx.wcount += 1
        for gi, g in enumerate(groups):
            sec = cx.wb[slot][:, secoff[gi]:secoff[gi] + g["nk"] * CB].rearrange("p (k c) -> p k c", c=CB)
            src = g["wap"](c0, cw).rearrange("(k p) n -> p k n", p=128)
            s.dma("pool", f_dma(sec[:, :, 0:cw], src), "wb%d_%d" % (slot, gi), writes=[("wb", slot, gi)])
        return slot
    slots = {}
    slots[0] = issue_w(0)
    if ncb > 1: slots[1] = issue_w(1)
    for cb in range(ncb):
        c0 = cb * CB; cw = min(CB, ncols - c0)
        slot = slots[cb]
        for fi in range(cw // 128):
            fo = fo_base + (c0 // 128) + fi
            outs = []
            for gi, g in enumerate(groups):
                sec = cx.wb[slot][:, secoff[gi]:secoff[gi] + g["nk"] * CB].rearrange("p (k c) -> p k c", c=CB)
                pt, pk = cx.next_ps()
                fns = [f_mm(pt[:], sec[:, kc, fi * 128:(fi + 1) * 128], g["rhs"](kc), kc == 0, kc == g["nk"] - 1)
                       for kc in range(g["nk"])]
                s.group("pe", fns, reads=[("wb", slot, gi)] + list(g["rkeys"]), writes=[pk])
                outs.append((pt, pk))
            epilogue(fo, outs)
        if cb + 2 < ncb:
            slots[cb + 2] = issue_w(cb + 2)

def emit_norm_prologue(cx, xsrc, t0, nwsb, hT, rstd, tag, x_from_sbuf=None):
    s = cx.s
    xTv = xsrc.rearrange("(kc p) t -> p kc t", p=128)
    pss = cx.ps[7]
    for kc in range(KC):
        xb = cx.xs[kc % 3]; sqb = cx.sq[kc % 2]
        s.dma("sp", f_dma(xb[:], xTv[:, kc, t0:t0 + TS]), "xs%d" % (kc % 3), writes=[("xs", kc % 3)])
        s.op("act", f_act(sqb[:], xb[:], AF.Square), reads=[("xs", kc % 3)], writes=[("sq", kc % 2)])
        s.op("dve", f_ts(hT[:, kc, :], xb[:], nwsb[:, kc:kc + 1], None, ALU.mult),
             reads=[("xs", kc % 3), "nw" + tag], writes=[("hT", kc)])
        s.group("pe", [f_mm(pss[:], cx.ones[:], sqb[:], kc == 0, kc == KC - 1)], reads=[("sq", kc % 2), "ones"], writes=[("ps", 7)])
    s.op("act", f_act(rstd[:], pss[:], AF.Sqrt, bias=EPS, scale=1.0 / D), reads=[("ps", 7)], writes=["rstd"])
    s.op("dve", f_recip(rstd[:], rstd[:]), reads=["rstd"], writes=["rstd"])

_BF_RANGES = ((2048, 3072), (4096, 7168), (9216, 11264), (13312, 13568))
def _row_map():
    m = {}; nf = 0; nb = 0
    for fo in range(NIN // 128):
        r = fo * 128
        if any(a <= r < b for a, b in _BF_RANGES):
            m[fo] = (True, nb); nb += 128
        else:
            m[fo] = (False, nf); nf += 128
    return m, nf, nb
_ROWMAP, NROW_F, NROW_B = _row_map()

def emit_phase_a(cx, xT, w, anw, out, outb):
    s = cx.s
    hT = cx.sb("a_hT", [128, KC, TS], BF16)
    cx.xs = [cx.sb("a_xs%d" % i, [128, TS], F32) for i in range(3)]
    cx.sq = [cx.sb("a_sq%d" % i, [128, TS], BF16) for i in range(2)]
    ob = [cx.sb("a_ob%d" % i, [128, TS], F32) for i in range(3)]
    obh = [cx.sb("a_obh%d" % i, [128, TS], BF16) for i in range(3)]
    rstd = cx.sb("a_rstd", [128, TS], F32)
    anw_sb = cx.sb("a_anw", [128, KC], F32)
    s.dma("sp", f_dma(anw_sb[:], anw), "anw", writes=["nwA"])
    cnt = [0]
    for st in range(TOK // TS):
        t0 = st * TS
        emit_norm_prologue(cx, xT, t0, anw_sb, hT, rstd, "A")
        def epi(fo, outs, t0=t0):
            pt, pk = outs[0]
            i = cnt[0] % 3; cnt[0] += 1
            isb, r0 = _ROWMAP[fo]
            if isb:
                s.op("dve", f_tt(obh[i][:], pt[:], rstd[:], ALU.mult), reads=[pk, "rstd"], writes=[("obh", i)])
                s.dma("sp", f_dma(outb[r0:r0 + 128, t0:t0 + TS], obh[i][:]), "obh%d" % i, reads=[("obh", i)])
            else:
                s.op("dve", f_tt(ob[i][:], pt[:], rstd[:], ALU.mult), reads=[pk, "rstd"], writes=[("ob", i)])
                s.dma("sp", f_dma(out[r0:r0 + 128, t0:t0 + TS], ob[i][:]), "ob%d" % i, reads=[("ob", i)])
        grp = dict(nk=KC, rhs=lambda kc: hT[:, kc, :], rkeys=[("hT", kc) for kc in range(KC)],
                   wap=lambda c0, cw: w[:, c0:c0 + cw])
        gemm(cx, [grp], NIN, 512, epi)

def build_phase_a():
    nc = bass.Bass("TRN2", target_bir_lowering=False)
    xT = nc.dram_tensor("xT", [D, TOK], F32, kind="ExternalInput").ap()
    w = nc.dram_tensor("w", [D, NIN], F32, kind="ExternalInput").ap()
    anw = nc.dram_tensor("anw", [128, KC], F32, kind="ExternalInput").ap()
    out = nc.dram_tensor("projT", [NROW_F, TOK], F32, kind="ExternalOutput").ap()
    outb = nc.dram_tensor("projTb", [NROW_B, TOK], BF16, kind="ExternalOutput").ap()
    with contextlib.ExitStack() as es:
        s = Sched(nc, es)
        cx = Ctx(nc, es, s)
        emit_phase_a(cx, xT, w, anw, out, outb)
        s.finish(); s.replay()
    return nc

HH = FFN // 2
NKH = HH // 128

def emit_phase_c(cx, yT, gdT, xT, wgu, bg, wbh, wbd, wbr, wo, fnw, wg, wu, wd, lnw, outT, last):
    s = cx.s
    bufA = cx.sb("bufA", [128, 32, TS], BF16)
    bufB = cx.sb("bufB", [128, NKH, TS], BF16)
    gdb = cx.sb("gdb", [128, 2, TS], BF16)
    cx.xs = [cx.sb("xs%d" % i, [128, TS], F32) for i in range(3)]
    cx.sq = [cx.sb("sq%d" % i, [128, TS], BF16) for i in range(2)]
    gt = [[cx.sb("gt%d_%d" % (i, j), [128, TS], F32) for j in range(3)] for i in range(2)]
    mt = [cx.sb("mt%d" % i, [128, TS], F32) for i in range(2)]
    tt = [cx.sb("tt%d" % i, [128, TS], F32) for i in range(2)]
    x1t = [cx.sb("x1t%d" % i, [128, TS], F32) for i in range(3)]
    tg = [cx.sb("tg%d" % i, [128, TS], F32) for i in range(2)]
    tsg = [cx.sb("tsg%d" % i, [128, TS], F32) for i in range(2)]
    tu = [cx.sb("tu%d" % i, [128, TS], F32) for i in range(2)]
    rstd = cx.sb("c_rstd", [128, TS], F32)
    bg_sb = cx.sb("bg_sb", [128, 96], F32)
    fnw_sb = cx.sb("fnw_sb", [128, KC], F32)
    lnw_sb = cx.sb("lnw_sb", [128, KC], F32)
    s.dma("sp", f_dma(bg_sb[:], bg), "bg", writes=["bg"])
    s.dma("sp", f_dma(fnw_sb[:], fnw), "fnw", writes=["fnw"])
    if last:
        s.dma("sp", f_dma(lnw_sb[:], lnw), "lnw", writes=["lnw"])
    yTv = yT.rearrange("(kc p) t -> p kc t", p=128)
    gdv = gdT.rearrange("(kc p) t -> p kc t", p=128)
    pss = cx.ps[7]
    cnt = {"x": 0, "p3": 0}
    for st in range(TOK // TS):
        t0 = st * TS
        for q in range(4):
            s.dma("pool", f_dma(bufA[:, q * 8:(q + 1) * 8, :], yTv[:, q * 8:(q + 1) * 8, t0:t0 + TS]), "yin%d" % q,
                  writes=[("bufA", kc) for kc in range(q * 8, (q + 1) * 8)])
        s.dma("pool", f_dma(gdb[:], gdv[:, :, t0:t0 + TS]), "gdin", writes=["gdb"])
        def epi1(fo, outs):
            i = fo % 2
            for br in range(3):
                pt, pk = outs[br]
                s.op("act", f_act(gt[i][br][:], pt[:], AF.Sigmoid, bias=bg_sb[:, br * 32 + fo:br * 32 + fo + 1]),
                     reads=[pk, "bg"], writes=[("gt", i, br)])
            s.op("dve", f_tt(mt[i][:], outs[3][0][:], gt[i][0][:], ALU.mult), reads=[outs[3][1], ("gt", i, 0)], writes=[("mt", i)])
            s.op("dve", f_tt(tt[i][:], outs[4][0][:], gt[i][1][:], ALU.mult), reads=[outs[4][1], ("gt", i, 1)], writes=[("tt", i)])
            s.op("pool", f_tt(mt[i][:], mt[i][:], tt[i][:], ALU.add), reads=[("mt", i), ("tt", i)], writes=[("mt", i)])
            s.op("dve", f_tt(tt[i][:], outs[5][0][:], gt[i][2][:], ALU.mult), reads=[outs[5][1], ("gt", i, 2), ("tt", i)], writes=[("tt", i)])
            s.op("pool", f_tt(bufB[:, fo, :], mt[i][:], tt[i][:], ALU.add), reads=[("mt", i), ("tt", i)], writes=[("bufB", fo)])
        groups = []
        for br in range(3):
            groups.append(dict(nk=2, rhs=lambda kc: gdb[:, kc, :], rkeys=["gdb"],
                               wap=(lambda c0, cw, br=br: wgu[:, br * D + c0: br * D + c0 + cw])))
        groups.append(dict(nk=8, rhs=lambda kc: bufA[:, kc, :], rkeys=[("bufA", k) for k in range(0, 8)], wap=lambda c0, cw: wbh[:, c0:c0 + cw]))
        groups.append(dict(nk=8, rhs=lambda kc: bufA[:, 8 + kc, :], rkeys=[("bufA", k) for k in range(8, 16)], wap=lambda c0, cw: wbd[:, c0:c0 + cw]))
        groups.append(dict(nk=16, rhs=lambda kc: bufA[:, 16 + kc, :], rkeys=[("bufA", k) for k in range(16, 32)], wap=lambda c0, cw: wbr[:, c0:c0 + cw]))
        gemm(cx, groups, D, 256, epi1)
        def epi2(fo, outs, t0=t0):
            pt, pk = outs[0]
            i = cnt["x"] % 3; cnt["x"] += 1
            s.dma("sp", f_dma(cx.xs[i][:], xT[fo * 128:(fo + 1) * 128, t0:t0 + TS]), "xs%d" % i, writes=[("xs", i)])
            s.op("dve", f_tt(x1t[i][:], pt[:], cx.xs[i][:], ALU.add), reads=[pk, ("xs", i)], writes=[("x1t", i)])
            s.dma("sp", f_dma(outT[fo * 128:(fo + 1) * 128, t0:t0 + TS], x1t[i][:]), "x1o%d" % i, reads=[("x1t", i)], writes=[("xacc", fo)])
            j = fo % 2
            s.op("act", f_act(cx.sq[j][:], x1t[i][:], AF.Square), reads=[("x1t", i)], writes=[("sq", j)])
            s.group("pe", [f_mm(pss[:], cx.ones[:], cx.sq[j][:], fo == 0, fo == KC - 1)], reads=[("sq", j), "ones"], writes=[("ps", 7)])
            s.op("pool", f_ts(bufA[:, fo, :], x1t[i][:], fnw_sb[:, fo:fo + 1], None, ALU.mult), reads=[("x1t", i), "fnw"], writes=[("bufA", fo)])
        grp = dict(nk=32, rhs=lambda kc: bufB[:, kc, :], rkeys=[("bufB", k) for k in range(32)], wap=lambda c0, cw: wo[:, c0:c0 + cw])
        gemm(cx, [grp], D, 512, epi2)
        s.op("act", f_act(rstd[:], pss[:], AF.Sqrt, bias=EPS, scale=1.0 / D), reads=[("ps", 7)], writes=["rstd"])
        s.op("dve", f_recip(rstd[:], rstd[:]), reads=["rstd"], writes=["rstd"])
        for hh in range(2):
            def epi3(fl, outs):
                i = cnt["p3"] % 2; cnt["p3"] += 1
                s.op("dve", f_tt(tg[i][:], outs[0][0][:], rstd[:], ALU.mult), reads=[outs[0][1], "rstd"], writes=[("tg", i)])
                s.op("act", f_act(tsg[i][:], tg[i][:], AF.Silu), reads=[("tg", i)], writes=[("tsg", i)])
                s.op("dve", f_tt(tu[i][:], outs[1][0][:], rstd[:], ALU.mult), reads=[outs[1][1], "rstd"], writes=[("tu", i)])
                s.op("pool", f_tt(bufB[:, fl, :], tsg[i][:], tu[i][:], ALU.mult), reads=[("tsg", i), ("tu", i)], writes=[("bufB", fl)])
            h0 = hh * HH
            g1 = dict(nk=32, rhs=lambda kc: bufA[:, kc, :], rkeys=[("bufA", k) for k in range(32)], wap=lambda c0, cw, h0=h0: wg[:, h0 + c0:h0 + c0 + cw])
            g2 = dict(nk=32, rhs=lambda kc: bufA[:, kc, :], rkeys=[("bufA", k) for k in range(32)], wap=lambda c0, cw, h0=h0: wu[:, h0 + c0:h0 + c0 + cw])
            gemm(cx, [g1, g2], HH, 256, epi3)
            fin = last and hh == 1
            def epi4(fo, outs, t0=t0, fin=fin):
                pt, pk = outs[0]
                i = cnt["x"] % 3; cnt["x"] += 1
                s.dma("sp", f_dma(cx.xs[i][:], outT[fo * 128:(fo + 1) * 128, t0:t0 + TS]), "xs%d" % i, reads=[("xacc", fo)], writes=[("xs", i)])
                s.op("dve", f_tt(x1t[i][:], pt[:], cx.xs[i][:], ALU.add), reads=[pk, ("xs", i)], writes=[("x1t", i)])
                s.dma("sp", f_dma(outT[fo * 128:(fo + 1) * 128, t0:t0 + TS], x1t[i][:]), "x1o%d" % i, reads=[("x1t", i)], writes=[("xacc", fo)])
                if fin:
                    j = fo % 2
                    s.op("act", f_act(cx.sq[j][:], x1t[i][:], AF.Square), reads=[("x1t", i)], writes=[("sq", j)])
                    s.group("pe", [f_mm(pss[:], cx.ones[:], cx.sq[j][:], fo == 0, fo == KC - 1)], reads=[("sq", j), "ones"], writes=[("ps", 7)])
            g4 = dict(nk=NKH, rhs=lambda kc: bufB[:, kc, :], rkeys=[("bufB", k) for k in range(NKH)], wap=lambda c0, cw, h0=h0: wd[h0:h0 + HH, c0:c0 + cw])
            gemm(cx, [g4], D, 256, epi4)
        if last:
            s.op("act", f_act(rstd[:], pss[:], AF.Sqrt, bias=EPS, scale=1.0 / D), reads=[("ps", 7)], writes=["rstd"])
            s.op("dve", f_recip(rstd[:], rstd[:]), reads=["rstd"], writes=["rstd"])
            for fo in range(KC):
                i = cnt["x"] % 3; cnt["x"] += 1
                s.dma("sp", f_dma(cx.xs[i][:], outT[fo * 128:(fo + 1) * 128, t0:t0 + TS]), "xs%d" % i, reads=[("xacc", fo)], writes=[("xs", i)])
                s.op("dve", f_stt(x1t[i][:], cx.xs[i][:], lnw_sb[:, fo:fo + 1], rstd[:], ALU.mult, ALU.mult), reads=[("xs", i), "lnw", "rstd"], writes=[("x1t", i)])
                s.dma("sp", f_dma(outT[fo * 128:(fo + 1) * 128, t0:t0 + TS], x1t[i][:]), "x1o%d" % i, reads=[("x1t", i)], writes=[("xacc", fo)])

def build_phase_c(last, with_a=False):
    nc = bass.Bass("TRN2", target_bir_lowering=False)
    di = lambda name, shape, dt=F32: nc.dram_tensor(name, shape, dt, kind="ExternalInput").ap()
    yT = di("yT", [D, TOK], BF16); gdT = di("gdT", [GR, TOK], BF16); xT = di("xT", [D, TOK])
    wgu = di("wgu", [GR, 3 * D]); bg = di("bg", [128, 96])
    wbh = di("wbh", [1024, D]); wbd = di("wbd", [1024, D]); wbr = di("wbr", [2048, D]); wo = di("wo", [D, D])
    fnw = di("fnw", [128, KC]); wg = di("wg", [D, FFN]); wu = di("wu", [D, FFN]); wd = di("wd", [FFN, D])
    lnw = di("lnw", [128, KC])
    outT = nc.dram_tensor("outT", [D, TOK], F32, kind="ExternalOutput").ap()
    if with_a:
        w = di("w", [D, NIN]); anw = di("anw", [128, KC])
        out = nc.dram_tensor("projT", [NROW_F, TOK], F32, kind="ExternalOutput").ap()
        outb = nc.dram_tensor("projTb", [NROW_B, TOK], BF16, kind="ExternalOutput").ap()
    with contextlib.ExitStack() as es:
        s = Sched(nc, es)
        cx = Ctx(nc, es, s)
        with contextlib.ExitStack() as es2:
            cx.es = es2
            emit_phase_c(cx, yT, gdT, xT, wgu, bg, wbh, wbd, wbr, wo, fnw, wg, wu, wd, lnw, outT, last)
            sched_barrier(s); sched_flush(s)
        if with_a:
            with contextlib.ExitStack() as es2:
                cx.es = es2
                emit_phase_a(cx, outT, w, anw, out, outb)
                sched_barrier(s); sched_flush(s)
        s.finish(); sched_flush(s)
    return nc

def sigmoid(v): return 1 / (1 + np.exp(-v))
def ref_phase_c(y, gd, x, wgu, bgv, wbh, wbd, wbr, wo, fnwv, wg, wu, wd, lnwv, last):
    gates = sigmoid(gd @ wgu + bgv)
    gh, gdd, gr = gates[:, :D], gates[:, D:2 * D], gates[:, 2 * D:]
    merged = gh * (y[:, :1024] @ wbh) + gdd * (y[:, 1024:2048] @ wbd) + gr * (y[:, 2048:] @ wbr)
    x1 = x + merged @ wo
    h = x1 / np.sqrt((x1 * x1).mean(-1, keepdims=True) + EPS) * fnwv
    g = h @ wg
    x2 = x1 + ((g * sigmoid(g)) * (h @ wu)) @ wd
    if last:
        x2 = x2 / np.sqrt((x2 * x2).mean(-1, keepdims=True) + EPS) * lnwv
    return x2


S = 8192; SEG = 1024; NCH = SEG // 32
CL = 32

def f_scan(out, d0, d1, init, op0, op1):
    return lambda e: e.tensor_tensor_scan(out=out, data0=d0, data1=d1, initial=init, op0=op0, op1=op1)
def f_reduce(out, in_, axis, op):
    return lambda e: e.tensor_reduce(out=out, in_=in_, axis=axis, op=op)
def f_tr(out, in_, ident):
    return lambda e: e.transpose(out, in_, ident)
def f_acta(out, in_, func, accum):
    return lambda e: e.activation(out=out, in_=in_, func=func, accum_out=accum)

def bc_chunk(ap2d, n):
    return ap2d.unsqueeze(2).to_broadcast([ap2d.shape[0], ap2d.shape[1], n])
def bc_mid(ap2d, m):
    return ap2d.unsqueeze(1).to_broadcast([ap2d.shape[0], m, ap2d.shape[1]])

class LA:
    def __init__(self, cx, tag, nh, dv, ps_a, ps_o, ps_t, ps_u, mask, ident):
        self.cx = cx; self.tag = tag; self.nh = nh; self.dv = dv
        self.ps_a, self.ps_o, self.ps_t, self.ps_u = ps_a, ps_o, ps_t, ps_u
        self.mask = mask; self.ident = ident
        sb = cx.sb
        self.S = [sb(tag + "S%d" % j, [128, dv], F32) for j in range(nh)]
        self.Sb = [sb(tag + "Sb%d" % j, [128, dv], BF16) for j in range(nh)]
        self.A = [[sb(tag + "A%d_%d" % (j, i), [CL, CL], BF16) for i in range(2)] for j in range(nh)]
        self.Kt = [[sb(tag + "Kk%d_%d" % (j, i), [CL, 128], BF16) for i in range(2)] for j in range(nh)]
        s = cx.s
        for j in range(nh):
            s.op("dve", f_memset(self.S[j][:], 0.0), writes=[(tag, "S", j)])
            s.op("dve", f_memset(self.Sb[j][:], 0.0), writes=[(tag, "Sb", j)])
        self.n = 0
    def run_segment(self, Qt, Kt, Qh, Kh, dec, V, O, keys):
        s = self.cx.s; tag = self.tag; dv = self.dv
        for c in range(NCH):
            cs = slice(c * CL, (c + 1) * CL)
            for j in range(self.nh):
                i = self.n % 2
                k = keys[j]
                pa = self.ps_a[j][i]; po = self.ps_o[j][i]; ptt = self.ps_t[j][i]; pu = self.ps_u[j]
                A = self.A[j][i]; Kk = self.Kt[j][i]
                s.group("pe", [f_mm(pa[:], Kt[j][:, cs], Qt[j][:, cs], True, True)], reads=[k["Kt"], k["Qt"]], writes=[(tag, "pa", j, i)])
                s.op("dve", f_tt(A[:], pa[:], self.mask[:], ALU.mult), reads=[(tag, "pa", j, i), "mask"], writes=[(tag, "A", j, i)])
                s.group("pe", [f_tr(ptt[:], Kh[j][:, cs], self.ident[:])], reads=[k["Kh"], "ident"], writes=[(tag, "pt", j, i)])
                s.op("act", f_copy_act(Kk[:], ptt[:]), reads=[(tag, "pt", j, i)], writes=[(tag, "Kk", j, i)])
                s.group("pe", [f_mm(po[:], A[:], V[j][:, c, :], True, False),
                               f_mm(po[:], Qh[j][:, cs], self.Sb[j][:], False, True)],
                        reads=[(tag, "A", j, i), k["V"], k["Qh"], (tag, "Sb", j)], writes=[(tag, "po", j, i)])
                s.op("act", f_copy_act(O[j][:, c, :], po[:]), reads=[(tag, "po", j, i)], writes=[k["O"]])
                s.group("pe", [f_mm(pu[:], Kk[:], V[j][:, c, :], True, True)], reads=[(tag, "Kk", j, i), k["V"]], writes=[(tag, "pu", j)])
                s.op("dve", f_stt(self.S[j][:], self.S[j][:], dec[j][:, c:c + 1], pu[:], ALU.mult, ALU.add),
                     reads=[(tag, "S", j), k["dec"], (tag, "pu", j)], writes=[(tag, "S", j)])
                s.op("act", f_copy_act(self.Sb[j][:], self.S[j][:]), reads=[(tag, "S", j)], writes=[(tag, "Sb", j)])
            self.n += 1

def la_psum(cx, dv):
    pb, pbt = cx.pb, cx.pbt
    ps_a = [[pb[0][0:CL, (2 * j + i) * CL:(2 * j + i + 1) * CL] for i in range(2)] for j in range(2)]
    ps_o = [[pb[1 + j][0:CL, i * 256:i * 256 + dv] for i in range(2)] for j in range(2)]
    ps_t = [[pbt[0:CL, (2 * j + i) * 128:(2 * j + i + 1) * 128] for i in range(2)] for j in range(2)]
    ps_u = [pb[4 + j][:, 0:dv] for j in range(2)]
    return ps_a, ps_o, ps_t, ps_u

def alloc_b_psum(cx):
    cx.pb = [cx.es.enter_context(cx.nc.psum_tensor("pb%d" % i, [128, 512], F32)) for i in range(7)]
    cx.pbt = cx.es.enter_context(cx.nc.psum_tensor("pbt", [128, 1024], BF16))

def f_copy_act(out, in_):
    return lambda e: e.copy(out=out, in_=in_)

def la_backend(cx, s, tag, j, O, g_src, nw_bc, ydst, dv, bufs, okey):
    gt, ssq, rs, yb = bufs
    s.op("act", f_act(gt[:], O[:], AF.Square), reads=[okey], writes=[(tag, "gt")])
    s.op("dve", f_reduce(ssq[:], gt[:], AX.X, ALU.add), reads=[(tag, "gt")], writes=[(tag, "ssq")])
    s.op("act", f_act(rs[:], ssq[:], AF.Sqrt, bias=EPS, scale=1.0 / dv), reads=[(tag, "ssq")], writes=[(tag, "rs")])
    s.op("dve", f_recip(rs[:], rs[:]), reads=[(tag, "rs")], writes=[(tag, "rs")])
    s.dma("sp", f_dma(gt[:], g_src), tag + "g", reads=[(tag, "gt")], writes=[(tag, "gt")])
    s.op("act", f_act(gt[:], gt[:], AF.Silu), reads=[(tag, "gt")], writes=[(tag, "gt")])
    s.op("dve", f_tt(gt[:], gt[:], bc_mid(nw_bc, NCH), ALU.mult), reads=[(tag, "gt"), "nw" + tag], writes=[(tag, "gt")])
    s.op("dve", f_tt(O[:], O[:], bc_chunk(rs[:], dv), ALU.mult), reads=[okey, (tag, "rs")], writes=[okey])
    s.op("dve", f_tt(yb[:], gt[:], O[:], ALU.mult), reads=[(tag, "gt"), okey], writes=[(tag, "yb")])
    s.dma("sp", f_dma(ydst, yb[:]), tag + "y", reads=[(tag, "yb")])

def emit_hgrn(cx, layer, hq, hf, hi, hg, lbl, hnw, yh, consts):
    s = cx.s; sb = cx.sb; nc = cx.nc; es = cx.es
    mask, ident, rst = consts["mask"], consts["ident"], consts["rst"]
    ps_a, ps_o, ps_t, ps_u = la_psum(cx, 128)
    la = LA(cx, "H", 2, 128, ps_a, ps_o, ps_t, ps_u, mask, ident)
    lb = []; oml = []
    for j in range(2):
        lg = sb("h_lg%d" % j, [128, 4], F32); ssum = sb("h_ls%d" % j, [128, 1], F32)
        lbj = sb("h_lb%d" % j, [128, 1], F32); omlj = sb("h_oml%d" % j, [128, 1], F32)
        s.dma("sp", f_dma(lg[:], lbl[j]), "hlb%d" % j, writes=[("lg", j)])
        s.op("act", f_act(lg[:], lg[:], AF.Exp), reads=[("lg", j)], writes=[("lg", j)])
        s.op("dve", f_reduce(ssum[:], lg[:], AX.X, ALU.add), reads=[("lg", j)], writes=[("ls", j)])
        s.op("dve", f_recip(ssum[:], ssum[:]), reads=[("ls", j)], writes=[("ls", j)])
        if layer == 0:
            s.op("dve", f_memset(lbj[:], 0.0), writes=[("lb", j)])
        else:
            s.op("dve", f_reduce(lbj[:], lg[:, 1:layer + 1], AX.X, ALU.add), reads=[("lg", j)], writes=[("lb", j)])
            s.op("dve", f_tt(lbj[:], lbj[:], ssum[:], ALU.mult), reads=[("lb", j), ("ls", j)], writes=[("lb", j)])
        s.op("dve", f_ts(omlj[:], lbj[:], -1.0, 1.0, ALU.mult, ALU.add), reads=[("lb", j)], writes=[("oml", j)])
        lb.append(lbj); oml.append(omlj)
    nw_bc = sb("h_nw", [CL, 128], F32)
    s.dma("sp", f_dma(nw_bc[:], hnw.partition_broadcast(CL)), "hnw", writes=["nwH"])
    W = {}
    for nm in ("q", "f", "b", "t1", "t2"):
        W[nm, 0] = W[nm, 1] = sb("h_%s" % nm, [128, SEG], F32)
    for j in range(2):
        for nm in ("Qt", "Kt", "Qh", "Kh"):
            W[nm, j] = sb("h_%s%d" % (nm, j), [128, SEG], BF16)
        W["dec", j] = sb("h_dec%d" % j, [128, NCH], F32)
        W["V", j] = sb("h_V%d" % j, [CL, NCH, 128], BF16)
        W["O", j] = sb("h_O%d" % j, [CL, NCH, 128], F32)
    bufs = (sb("h_gt", [CL, NCH, 128], F32), sb("h_ssq", [CL, NCH], F32), sb("h_rs", [CL, NCH], F32), sb("h_yb", [CL, NCH, 128], BF16))
    sc = 128 ** -0.5
    for seg in range(S // SEG):
        t0 = seg * SEG
        keys = []
        for j in range(2):
            q, f, b, t1, t2 = (W[n, j] for n in ("q", "f", "b", "t1", "t2"))
            K = lambda n, j=j: ("H", n, j) if n in ("Qt", "Kt", "Qh", "Kh", "dec", "V", "O") else ("H", n)
            s.dma("sp", f_dma(q[:], hq[j][:, t0:t0 + SEG]), "hq%d" % j, writes=[K("q")])
            s.dma("sp", f_dma(f[:], hf[j][:, t0:t0 + SEG]), "hf%d" % j, writes=[K("f")])
            s.dma("pool", f_dma(W["V", j][:], hi[j][t0:t0 + SEG, :].rearrange("(c p) e -> p c e", p=CL)), "hV%d" % j, writes=[K("V")])
            s.op("act", f_act(q[:], q[:], AF.Silu), reads=[K("q")], writes=[K("q")])
            s.op("act", f_act(f[:], f[:], AF.Sigmoid), reads=[K("f")], writes=[K("f")])
            s.op("dve", f_ts(f[:], f[:], oml[j][:], lb[j][:], ALU.mult, ALU.add), reads=[K("f"), ("oml", j), ("lb", j)], writes=[K("f")])
            s.op("act", f_act(t1[:], f[:], AF.Ln), reads=[K("f")], writes=[K("t1")])
            s.op("dve", f_scan(b[:], rst[:], t1[:], 0.0, ALU.mult, ALU.add), reads=[K("t1"), "rst"], writes=[K("b")])
            s.op("pool", f_ts(f[:], f[:], -1.0, 1.0, ALU.mult, ALU.add), reads=[K("f")], writes=[K("f")])
            b3 = b[:].rearrange("p (c t) -> p c t", t=CL)
            bl = b3[:, :, CL - 1]; br = b3[:, :, CL // 2 - 1]
            s.op("act", f_act(W["dec", j][:], bl, AF.Exp), reads=[K("b")], writes=[K("dec")])
            s.op("act", f_act(t1[:], b[:], AF.Exp), reads=[K("b"), K("t1")], writes=[K("t1")])
            s.op("dve", f_stt(W["Qh", j][:], q[:], sc, t1[:], ALU.mult, ALU.mult), reads=[K("q"), K("t1")], writes=[K("Qh")])
            t13 = t1[:].rearrange("p (c t) -> p c t", t=CL); t23 = t2[:].rearrange("p (c t) -> p c t", t=CL)
            s.op("dve", f_tt(t23, bc_chunk(bl, CL), b3, ALU.subtract), reads=[K("b"), K("t2")], writes=[K("t2")])
            s.op("act", f_act(t2[:], t2[:], AF.Exp), reads=[K("t2")], writes=[K("t2")])
            s.op("pool", f_tt(W["Kh", j][:], f[:], t2[:], ALU.mult), reads=[K("f"), K("t2")], writes=[K("Kh")])
            s.op("dve", f_tt(t13, b3, bc_chunk(br, CL), ALU.subtract), reads=[K("b"), K("t1"), K("Qh")], writes=[K("t1")])
            s.op("dve", f_ts(t2[:], t1[:], -1.0, 40.0, ALU.mult, ALU.min), reads=[K("t1"), K("Kh")], writes=[K("t2")])
            s.op("pool", f_ts(t1[:], t1[:], 40.0, None, ALU.min), reads=[K("t1")], writes=[K("t1")])
            s.op("act", f_act(t1[:], t1[:], AF.Exp), reads=[K("t1")], writes=[K("t1")])
            s.op("act", f_act(t2[:], t2[:], AF.Exp), reads=[K("t2")], writes=[K("t2")])
            s.op("dve", f_stt(W["Qt", j][:], q[:], sc, t1[:], ALU.mult, ALU.mult), reads=[K("q"), K("t1")], writes=[K("Qt")])
            s.op("pool", f_tt(W["Kt", j][:], f[:], t2[:], ALU.mult), reads=[K("f"), K("t2")], writes=[K("Kt")])
            keys.append({n: K(n) for n in ("Qt", "Kt", "Qh", "Kh", "dec", "V", "O")})
        la.run_segment([W["Qt", j] for j in range(2)], [W["Kt", j] for j in range(2)], [W["Qh", j] for j in range(2)],
                       [W["Kh", j] for j in range(2)], [W["dec", j] for j in range(2)], [W["V", j] for j in range(2)],
                       [W["O", j] for j in range(2)], keys)
        for j in range(2):
            la_backend(cx, s, "H", j, W["O", j], hg[j][t0:t0 + SEG, :].rearrange("(c p) e -> p c e", p=CL), nw_bc[:],
                       yh[j][t0:t0 + SEG, :].rearrange("(c p) e -> p c e", p=CL), 128, bufs, ("H", "O", j))

def emit_ret(cx, rq, rk, cos2, sin2, rv, rg, rnw, rtab, yr):
    s = cx.s; sb = cx.sb; nc = cx.nc; es = cx.es
    mask, ident = cx.consts["mask"], cx.consts["ident"]
    ps_a, ps_o, ps_t, ps_u = la_psum(cx, 256)
    la = LA(cx, "R", 2, 256, ps_a, ps_o, ps_t, ps_u, mask, ident)
    nw_bc = sb("r_nw", [CL, 256], F32)
    s.dma("sp", f_dma(nw_bc[:], rnw.partition_broadcast(CL)), "rnw", writes=["nwR"])
    tab = [sb("r_tab%d" % j, [128, 5, CL], F32) for j in range(2)]
    dec = [sb("r_dec%d" % j, [128, NCH], F32) for j in range(2)]
    for j in range(2):
        s.dma("sp", f_dma(tab[j][:], rtab[j]), "rtab%d" % j, writes=[("rtab", j)])
        s.op("dve", f_copy(dec[j][:].rearrange("p (a c) -> p a c", c=CL), bc_mid(tab[j][:, 4, :], NCH // CL)), reads=[("rtab", j)], writes=[("R", "dec", j)])
    cs = sb("r_cos", [128, SEG], F32); sn = sb("r_sin", [128, SEG], F32)
    W = {}
    for nm in ("a", "as", "qr", "kr"):
        W[nm, 0] = W[nm, 1] = sb("r_%s" % nm, [128, SEG], F32)
    for j in range(2):
        for nm in ("Qt", "Kt", "Qh", "Kh"):
            W[nm, j] = sb("r_%s%d" % (nm, j), [128, SEG], BF16)
        W["V", j] = sb("r_V%d" % j, [CL, NCH, 256], BF16)
        W["O", j] = sb("r_O%d" % j, [CL, NCH, 256], F32)
    bufs = (sb("r_gt", [CL, NCH, 256], F32), sb("r_ssq", [CL, NCH], F32), sb("r_rs", [CL, NCH], F32), sb("r_yb", [CL, NCH, 256], BF16))
    for seg in range(S // SEG):
        t0 = seg * SEG
        s.dma("sp", f_dma(cs[:], cos2[:, t0:t0 + SEG]), "rcos", writes=["rcos"])
        s.dma("sp", f_dma(sn[:], sin2[:, t0:t0 + SEG]), "rsin", writes=["rsin"])
        keys = []
        for j in range(2):
            K = lambda n, j=j: ("R", n, j) if n in ("Qt", "Kt", "Qh", "Kh", "dec", "V", "O") else ("R", n)
            a, as_, qr, kr = (W[n, j] for n in ("a", "as", "qr", "kr"))
            for (src, dst, dk) in ((rq, qr, "qr"), (rk, kr, "kr")):
                s.dma("sp", f_dma(a[:], src[j][:, t0:t0 + SEG]), "ra%d" % j, writes=[K("a")])
                s.dma("sp", f_dma(as_[0:64, :], src[j][64:128, t0:t0 + SEG]), "ras%d" % j, writes=[K("as")])
                s.dma("sp", f_dma(as_[64:128, :], src[j][0:64, t0:t0 + SEG]), "rasb%d" % j, writes=[K("as")])
                s.op("dve", f_tt(a[:], a[:], cs[:], ALU.mult), reads=[K("a"), "rcos"], writes=[K("a")])
                s.op("pool", f_tt(as_[:], as_[:], sn[:], ALU.mult), reads=[K("as"), "rsin"], writes=[K("as")])
                s.op("dve", f_tt(dst[:], a[:], as_[:], ALU.add), reads=[K("a"), K("as")], writes=[K(dk)])
            s.dma("pool", f_dma(W["V", j][:], rv[j][t0:t0 + SEG, :].rearrange("(c p) e -> p c e", p=CL)), "rV%d" % j, writes=[K("V")])
            for ti, (nm, srcb, eng) in enumerate((("Qt", qr, "dve"), ("Kt", kr, "pool"), ("Qh", qr, "dve"), ("Kh", kr, "pool"))):
                s.op(eng, f_tt(W[nm, j][:].rearrange("p (c t) -> p c t", t=CL), srcb[:].rearrange("p (c t) -> p c t", t=CL),
                               bc_mid(tab[j][:, ti, :], NCH), ALU.mult), reads=[K("qr" if srcb is qr else "kr"), ("rtab", j)], writes=[K(nm)])
            keys.append({n: K(n) for n in ("Qt", "Kt", "Qh", "Kh", "dec", "V", "O")})
        la.run_segment([W["Qt", j] for j in range(2)], [W["Kt", j] for j in range(2)], [W["Qh", j] for j in range(2)],
                       [W["Kh", j] for j in range(2)], dec, [W["V", j] for j in range(2)], [W["O", j] for j in range(2)], keys)
        for j in range(2):
            la_backend(cx, s, "R", j, W["O", j], rg[j][t0:t0 + SEG, :].rearrange("(c p) e -> p c e", p=CL), nw_bc[:],
                       yr[j][t0:t0 + SEG, :].rearrange("(c p) e -> p c e", p=CL), 256, bufs, ("R", "O", j))

def load_consts(cx, mask_d, ident_d, rst_d):
    s = cx.s
    mask = cx.sb("c_mask", [CL, CL], F32); ident = cx.sb("c_ident", [128, 128], BF16); rst = cx.sb("c_rst", [128, SEG], F32)
    s.dma("sp", f_dma(mask[:], mask_d), "cmask", writes=["mask"])
    s.dma("pool", f_dma(ident[:], ident_d), "cident", writes=["ident"])
    s.dma("sp", f_dma(rst[:], rst_d), "crst", writes=["rst"])
    cx.consts = {"mask": mask, "ident": ident, "rst": rst}
    return cx.consts

def host_consts():
    mask = np.triu(np.ones((CL, CL), np.float32))
    ident = np.eye(128, dtype=np.float32)
    rst = np.ones((128, SEG), np.float32); rst[:, ::CL] = 0.0
    return mask, ident, rst

def host_ret_tables(head):
    lg = np.log(1.0 - 2.0 ** (-5.0 - head))
    t = np.arange(CL, dtype=np.float64)
    sc = 128 ** -0.5
    tab = np.stack([np.exp(lg * (t - 15)), sc * np.exp(lg * (15 - t)), np.exp(lg * (t + 1)), sc * np.exp(lg * (31 - t)),
                    np.full(CL, np.exp(lg * CL))], 0)
    return np.broadcast_to(tab[None], (128, 5, CL)).astype(np.float32).copy()

def host_rope_tables():
    half = 64
    inv = (1.0 / (10000.0 ** (np.arange(half, dtype=np.float32) / half))).astype(np.float32)
    ang = (np.arange(S, dtype=np.float32)[:, None] * inv[None, :]).astype(np.float32)
    cos, sin = np.cos(ang).astype(np.float32).T, np.sin(ang).astype(np.float32).T
    return np.concatenate([cos, cos], 0).copy(), np.concatenate([-sin, sin], 0).copy()

QS = 256; NQS = S // QS; NKB = S // 128

def host_diff_consts():
    k = np.arange(128)[:, None]; q = np.arange(128)[None, :]
    return np.concatenate([(q - k), (128 + q - k)], 1).astype(np.float32)

def t5_steps():
    n = np.arange(0, 256)
    nf = np.maximum(n, 1).astype(np.float32)
    large = 16 + (np.log(nf / np.float32(16)) / np.float32(np.log(128 / 16)) * np.float32(16)).astype(np.int32)
    large = np.minimum(large, 31)
    b = np.where(n < 16, n, large)
    steps = [(int(i), int(b[i]), int(b[i - 1])) for i in range(1, 256) if b[i] != b[i - 1]]
    return int(b[0]), steps

def emit_diff(cx, layer, dq, dk, dv_, relb, dlam, dnw, dmat, yd, stage=1):
    import math
    s = cx.s; sb = cx.sb
    lam_init = 0.8 - 0.6 * math.exp(-0.3 * layer)
    pb = cx.pb
    dl = sb("d_dl", [128, 256], F32); pr = sb("d_pr", [128, 128], F32); s12 = sb("d_s12", [128, 2], F32); nlam = sb("d_nlam", [128, 1], F32)
    s.dma("sp", f_dma(dl[:], dlam.partition_broadcast(128)), "ddl", writes=["dl"])
    dl3 = dl[:].rearrange("p (a d) -> p a d", d=64)
    s.op("dve", f_tt(pr[:].rearrange("p (a d) -> p a d", d=64), dl3[:, 0::2, :], dl3[:, 1::2, :], ALU.mult), reads=["dl"], writes=["pr"])
    s.op("dve", f_reduce(s12[:], pr[:].rearrange("p (a d) -> p a d", d=64), AX.X, ALU.add), reads=["pr"], writes=["s12"])
    s.op("act", f_act(s12[:], s12[:], AF.Exp), reads=["s12"], writes=["s12"])
    s.op("dve", f_tt(nlam[:], s12[:, 1:2], s12[:, 0:1], ALU.subtract), reads=["s12"], writes=["nlam"])
    s.op("dve", f_ts(nlam[:], nlam[:], -lam_init, None, ALU.add), reads=["nlam"], writes=["nlam"])
    nw = sb("d_nw", [128, 128], F32)
    s.dma("sp", f_dma(nw[:], dnw.partition_broadcast(128)), "dnw", writes=["dnw"])
    s.op("dve", f_ts(nw[:], nw[:], 1.0 - lam_init, None, ALU.mult), reads=["dnw"], writes=["dnw"])
    dm = sb("d_dm", [128, 256], F32)
    s.dma("sp", f_dma(dm[:], dmat), "ddm", writes=["dm"])
    b0, steps = t5_steps()
    E = []
    for j in range(2):
        rb = sb("d_rb%d" % j, [128, 32], F32); acc = sb("d_acc%d" % j, [128, 256], F32); tmp = sb("d_tmp%d" % j, [128, 256], F32)
        dlt = sb("d_dlt%d" % j, [128, 1], F32); Ej = sb("d_E%d" % j, [128, 256], BF16)
        s.dma("sp", f_dma(rb[:], relb[j].partition_broadcast(128)), "drb%d" % j, writes=[("rb", j)])
        s.op("dve", f_tt(dlt[:], rb[:, b0:b0 + 1], rb[:, 31:32], ALU.subtract), reads=[("rb", j)], writes=[("dlt", j)])
        s.op("dve", f_ts(acc[:], dm[:], 0.0, dlt[:], ALU.mult, ALU.add), reads=["dm", ("dlt", j)], writes=[("acc", j)])
        for (n, bn, bo) in steps:
            s.op("dve", f_tt(dlt[:], rb[:, bn:bn + 1], rb[:, bo:bo + 1], ALU.subtract), reads=[("rb", j), ("dlt", j)], writes=[("dlt", j)])
            s.op("dve", f_ts(tmp[:], dm[:], float(n), dlt[:], ALU.is_ge, ALU.mult), reads=["dm", ("dlt", j)], writes=[("tmp", j)])
            s.op("dve", f_tt(acc[:], acc[:], tmp[:], ALU.add), reads=[("acc", j), ("tmp", j)], writes=[("acc", j)])
        s.op("act", f_act(acc[:], acc[:], AF.Exp), reads=[("acc", j)], writes=[("acc", j)])
        s.op("dve", f_ts(tmp[:], dm[:], 0.0, None, ALU.is_ge), reads=["dm"], writes=[("tmp", j)])
        s.op("dve", f_tt(Ej[:], acc[:], tmp[:], ALU.mult), reads=[("acc", j), ("tmp", j)], writes=[("E", j)])
        E.append(Ej)
    if stage == 0:
        dbg = sb("d_dbg", [128, 256], F32)
        s.op("dve", f_copy(dbg[:], E[0][:]), reads=[("E", 0)], writes=["dbg"])
        s.dma("sp", f_dma(yd[0][0:128, :], dbg[:, 0:128]), "dbg0", reads=["dbg"])
        s.dma("sp", f_dma(yd[0][128:256, :], dbg[:, 128:256]), "dbg1", reads=["dbg"])
        s.op("dve", f_ts(dbg[:, 0:128], dm[:, 0:128], 0.0, nlam[:], ALU.mult, ALU.add), reads=["dbg", "nlam", "dm"], writes=["dbg2"]); s.dma("sp", f_dma(yd[1][0:128, :], dbg[:, 0:128]), "dbg2", reads=["dbg2"])
        s.dma("sp", f_dma(yd[1][128:256, :], nw[:]), "dbg3", reads=["dnw"])
        return
    QTa = sb("d_QTa", [128, S], BF16); QTb = sb("d_QTb", [128, S], BF16); KT = sb("d_KT", [128, S], BF16); VX = sb("d_VX", [128, NKB, 132], BF16)
    P = [sb("d_P%d" % i, [128, 512], BF16) for i in range(3)]
    ut = [sb("d_u%d" % i, [128, 128], F32) for i in range(2)]; tt_ = [sb("d_t%d" % i, [128, 128], F32) for i in range(2)]
    yt = [sb("d_y%d" % i, [128, 128], BF16) for i in range(2)]; junk = sb("d_junk", [128, 128], F32)
    sm = [sb("d_sm%d" % i, [128, 4], F32) for i in range(2)]
    ps_s = [pb[0], pb[1], pb[2]]
    ps_o = [[pb[3][:, 0:129], pb[4][:, 0:129]], [pb[5][:, 0:129], pb[6][:, 0:129]]]
    s.op("dve", f_memset(VX[:, :, 128:129], 1.0), writes=["VXone"])
    s.op("dve", f_memset(QTa[64:128, :], 0.0), writes=["QTz"])
    s.op("pool", f_memset(QTb[0:64, :], 0.0), writes=["QTz2"])
    it = 0; fin = 0
    for j in range(2):
        for q4 in range(4):
            sl = slice(q4 * (S // 4), (q4 + 1) * (S // 4))
            s.dma("pool", f_dma(QTa[0:64, sl], dq[j][0:64, sl]), "dQT", writes=["QT"])
            s.dma("pool", f_dma(QTb[64:128, sl], dq[j][64:128, sl]), "dQT2", writes=["QT"])
            s.dma("pool", f_dma(KT[:, sl], dk[j][:, sl]), "dKT", writes=["KT"])
        s.dma("pool", f_dma(VX[:, :, 0:128], dv_[j].rearrange("(kb p) e -> p kb e", p=128)), "dVX", writes=["VX"])
        its = [(qs, kb) for qs in range(NQS) for kb in range(2 * qs + 2)]
        it0 = it
        def emit_S(n, j=j, it0=it0):
            qs, kb = its[n]; i = (it0 + n) % 3
            qsl = slice(qs * QS, (qs + 1) * QS); ksl = slice(kb * 128, (kb + 1) * 128)
            pss = ps_s[i]
            s.group("pe", [f_mm(pss[:, 0:256], KT[:, ksl], QTa[:, qsl], True, True),
                           f_mm(pss[:, 256:512], KT[:, ksl], QTb[:, qsl], True, True)],
                    reads=["QT", "KT", "QTz", "QTz2"], writes=[("pss", i)])
        def emit_rest(n, j=j, it0=it0):
            nonlocal fin
            qs, kb = its[n]; i = (it0 + n) % 3
            qb0 = 2 * qs; qb1 = qb0 + 1
            pss = ps_s[i]; Pt = P[i]
            s.op("act", f_act(Pt[:], pss[:], AF.Exp, scale=0.125), reads=[("pss", i)], writes=[("P", i)])
            P3 = Pt[:].rearrange("p (m q) -> p m q", m=2)
            def mul(qoff, eoff):
                s.op("dve", f_tt(P3[:, :, qoff:qoff + 128], P3[:, :, qoff:qoff + 128], bc_mid(E[j][:, eoff:eoff + 128], 2), ALU.mult),
                     reads=[("P", i), ("E", j)], writes=[("P", i)])
            jqs = [0, 1]
            if kb == qb0 - 1: mul(0, 128)
            if kb == qb0: mul(0, 0); mul(128, 128)
            if kb == qb1: mul(128, 0); jqs = [1]
            fns = []; wr = []
            for jq in jqs:
                last = qb0 if jq == 0 else qb1
                for m in range(2):
                    fns.append(f_mm(ps_o[jq][m], Pt[:, m * 256 + jq * 128:m * 256 + jq * 128 + 128], VX[:, kb, 0:129], kb == 0, kb == last))
                    wr.append(("pso", jq, m))
            s.group("pe", fns, reads=[("P", i), "VX", "VXone"], writes=wr)
            for jq in jqs:
                last = qb0 if jq == 0 else qb1
                if kb != last: continue
                f = fin % 2; fin += 1
                O1, O2 = ps_o[jq][0], ps_o[jq][1]
                k1, k2 = ("pso", jq, 0), ("pso", jq, 1)
                s.op("dve", f_recip(sm[f][:, 0:1], O1[:, 128:129]), reads=[k1], writes=[("sm", f)])
                s.op("dve", f_recip(sm[f][:, 1:2], O2[:, 128:129]), reads=[k2, ("sm", f)], writes=[("sm", f)])
                s.op("dve", f_tt(sm[f][:, 1:2], sm[f][:, 1:2], nlam[:], ALU.mult), reads=[("sm", f), "nlam"], writes=[("sm", f)])
                s.op("act", f_act(tt_[f][:], O1[:, 0:128], AF.Copy, scale=sm[f][:, 0:1]), reads=[k1, ("sm", f)], writes=[("tt", f)])
                s.op("dve", f_stt(ut[f][:], O2[:, 0:128], sm[f][:, 1:2], tt_[f][:], ALU.mult, ALU.add), reads=[k2, ("sm", f), ("tt", f)], writes=[("ut", f)])
                s.op("act", f_act(junk[:], ut[f][:], AF.Square), reads=[("ut", f)], writes=["junk"])
                s.op("dve", f_reduce(sm[f][:, 2:3], junk[:], AX.X, ALU.add), reads=["junk", ("sm", f)], writes=[("sm", f)])
                s.op("act", f_act(sm[f][:, 3:4], sm[f][:, 2:3], AF.Sqrt, bias=EPS, scale=1.0 / 128), reads=[("sm", f)], writes=[("sm", f)])
                s.op("dve", f_recip(sm[f][:, 3:4], sm[f][:, 3:4]), reads=[("sm", f)], writes=[("sm", f)])
                s.op("dve", f_stt(yt[f][:], ut[f][:], sm[f][:, 3:4], nw[:], ALU.mult, ALU.mult), reads=[("ut", f), ("sm", f), "dnw"], writes=[("yt", f)])
                r0 = (qb0 + jq) * 128
                s.dma("sp", f_dma(yd[j][r0:r0 + 128, :], yt[f][:]), "dy%d" % f, reads=[("yt", f)])
        PRE = 2
        for n in range(min(PRE, len(its))):
            emit_S(n)
        for n in range(len(its)):
            if n + PRE < len(its):
                emit_S(n + PRE)
            emit_rest(n)
        it += len(its)


DEPTH = 4
_PROGS = {}

def _prog_a():
    if "A" not in _PROGS:
        _PROGS["A"] = build_phase_a()
    return _PROGS["A"]

def _prog_c(last, with_a):
    k = ("C", bool(last), bool(with_a))
    if k not in _PROGS:
        _PROGS[k] = build_phase_c(bool(last), bool(with_a))
    return _PROGS[k]

def _prog_b(layer):
    k = ("B", layer)
    if k in _PROGS:
        return _PROGS[k]
    nc = bass.Bass("TRN2", target_bir_lowering=False)
    di = lambda name, shape, dt=F32: nc.dram_tensor(name, shape, dt, kind="ExternalInput").ap()
    do = lambda name, shape: nc.dram_tensor(name, shape, BF16, kind="ExternalOutput").ap()
    mask_d = di("mask", [CL, CL]); ident_d = di("ident", [128, 128]); rst_d = di("rst", [128, SEG])
    hq = di("hq", [2, 128, S]); hf = di("hf", [2, 128, S]); hi = di("hi", [2, S, 128], BF16); hg = di("hg", [2, S, 128])
    lbl = di("lbl", [2, 128, 4]); hnw = di("hnw", [128]); yh = do("yh", [2, S, 128])
    rq = di("rq", [2, 128, S]); rk = di("rk", [2, 128, S])
    cos2 = di("cos2", [128, S]); sin2 = di("sin2", [128, S]); rv = di("rv", [2, S, 256], BF16); rg = di("rg", [2, S, 256])
    rnw = di("rnw", [256]); rtab = di("rtab", [2, 128, 5, CL]); yr = do("yr", [2, S, 256])
    dq = di("dq", [2, 128, S], BF16); dk = di("dk", [2, 128, S], BF16); dv_ = di("dv", [2, S, 128], BF16); relb = di("relb", [2, 32])
    dlam = di("dlam", [256]); dnw = di("dnw", [128]); dmat = di("dmat", [128, 256]); yd = do("yd", [2, S, 128])
    with contextlib.ExitStack() as es0:
        s = Sched(nc, es0)
        with contextlib.ExitStack() as es:
            cx = Ctx(nc, es, s, gemm=False); alloc_b_psum(cx)
            load_consts(cx, mask_d, ident_d, rst_d)
            with contextlib.ExitStack() as es2:
                cx.es = es2
                emit_hgrn(cx, layer, hq, hf, hi, hg, lbl, hnw, yh, cx.consts)
                sched_barrier(s); sched_flush(s)
            with contextlib.ExitStack() as es2:
                cx.es = es2
                emit_ret(cx, rq, rk, cos2, sin2, rv, rg, rnw, rtab, yr)
                sched_barrier(s); sched_flush(s)
            with contextlib.ExitStack() as es2:
                cx.es = es2
                emit_diff(cx, layer, dq, dk, dv_, relb, dlam, dnw, dmat, yd)
                sched_barrier(s); sched_flush(s)
            s.finish(); sched_flush(s)
    _PROGS[k] = nc
    return nc

_OFF = {"hq": 0, "hf": 1024, "hi": 2048, "hg": 3072, "dq": 4096, "dk": 5120, "dv": 6144,
        "rq": 7168, "rk": 8192, "rv": 9216, "rg": 11264, "gd": 13312}

def _lay(v):
    return np.ascontiguousarray(np.asarray(v, np.float32).reshape(-1, 128).T)

def _rows(PTf, PTb, name, r0, n):
    a = _OFF[name] + r0
    isb, o = _ROWMAP[a // 128]
    o += a % 128
    return (PTb if isb else PTf)[o:o + n, :]

def kernel(x, attn_norm_w, w_in, lb_logits, hgrn_norm_w, rel_bias, diff_lambda, diff_norm_w, ret_norm_w, w_gate_up,
           b_gate, w_br_hgrn, w_br_diff, w_br_ret, w_o, ffn_norm_w, w_ffn_gate, w_ffn_up, w_ffn_down, final_norm_w):
    f32 = lambda a: np.asarray(a, np.float32)
    x = f32(x)
    NCORE = 8; cores = list(range(NCORE))
    xT = [np.ascontiguousarray(x[c // 4, (c % 4) * TOK:(c % 4 + 1) * TOK, :].T) for c in cores]
    mask, ident, rst = host_consts()
    cos2, sin2 = host_rope_tables()
    dmat = host_diff_consts()
    rtabs = [host_ret_tables(h) for h in range(8)]
    lbl_all = f32(lb_logits)
    res = run_bass_kernel_spmd(_prog_a(), [{"xT": xT[c], "w": f32(w_in[0]), "anw": _lay(attn_norm_w[0])} for c in cores], core_ids=cores)
    pf = [res.results[c]["projT"] for c in cores]; pb_ = [res.results[c]["projTb"] for c in cores]
    del res
    for l in range(DEPTH):
        PTf = [np.concatenate(pf[b * 4:(b + 1) * 4], axis=1) for b in range(2)]
        PTb = [np.concatenate(pb_[b * 4:(b + 1) * 4], axis=1) for b in range(2)]
        gdT = [np.ascontiguousarray(_rows(pf[c], pb_[c], "gd", 0, GR)) for c in cores]
        del pf, pb_
        in_b = []
        for c in cores:
            b, hp = c // 4, c % 4
            fm = lambda name, d: np.ascontiguousarray(_rows(PTf[b], PTb[b], name, 2 * hp * d, 2 * d).reshape(2, d, S))
            tm = lambda name, d: np.ascontiguousarray(_rows(PTf[b], PTb[b], name, 2 * hp * d, 2 * d).reshape(2, d, S).transpose(0, 2, 1))
            in_b.append({
                "mask": mask, "ident": ident, "rst": rst,
                "hq": fm("hq", 128), "hf": fm("hf", 128), "hi": tm("hi", 128), "hg": tm("hg", 128),
                "lbl": np.ascontiguousarray(lbl_all[:, 2 * hp * 128:2 * (hp + 1) * 128].T.reshape(2, 128, 4)),
                "hnw": f32(hgrn_norm_w[l]),
                "rq": fm("rq", 128), "rk": fm("rk", 128), "cos2": cos2, "sin2": sin2,
                "rv": tm("rv", 256), "rg": tm("rg", 256), "rnw": f32(ret_norm_w[l]),
                "rtab": np.stack([rtabs[2 * hp], rtabs[2 * hp + 1]], 0),
                "dq": fm("dq", 128), "dk": fm("dk", 128), "dv": tm("dv", 128),
                "relb": np.ascontiguousarray(f32(rel_bias)[:, 2 * hp:2 * hp + 2].T),
                "dlam": np.ascontiguousarray(f32(diff_lambda[l]).reshape(256)), "dnw": f32(diff_norm_w[l]), "dmat": dmat,
            })
        del PTf, PTb
        res = run_bass_kernel_spmd(_prog_b(l), in_b, core_ids=cores)
        del in_b
        ydt = res.results[0]["yh"].dtype
        Y = [np.empty((S, D), ydt) for _ in range(2)]
        for c in cores:
            b, hp = c // 4, c % 4
            r = res.results[c]
            for j in range(2):
                h = 2 * hp + j
                Y[b][:, h * 128:(h + 1) * 128] = r["yh"][j]
                Y[b][:, 1024 + h * 128:1024 + (h + 1) * 128] = r["yd"][j]
                Y[b][:, 2048 + h * 256:2048 + (h + 1) * 256] = r["yr"][j]
        del res
        yT = [np.ascontiguousarray(Y[c // 4][(c % 4) * TOK:(c % 4 + 1) * TOK, :].T) for c in cores]
        del Y
        last = (l == DEPTH - 1)
        wts = {"wgu": f32(w_gate_up[l]), "bg": _lay(b_gate[l]), "wbh": f32(w_br_hgrn[l]), "wbd": f32(w_br_diff[l]),
               "wbr": f32(w_br_ret[l]), "wo": f32(w_o[l]), "fnw": _lay(ffn_norm_w[l]), "wg": f32(w_ffn_gate[l]),
               "wu": f32(w_ffn_up[l]), "wd": f32(w_ffn_down[l]), "lnw": _lay(final_norm_w)}
        if not last:
            wts["w"] = f32(w_in[l + 1]); wts["anw"] = _lay(attn_norm_w[l + 1])
        res = run_bass_kernel_spmd(_prog_c(last, not last), [dict(wts, yT=yT[c], gdT=gdT[c], xT=xT[c]) for c in cores], core_ids=cores)
        xT = [res.results[c]["outT"] for c in cores]
        if not last:
            pf = [res.results[c]["projT"] for c in cores]; pb_ = [res.results[c]["projTb"] for c in cores]
        del res, yT, gdT, wts
    out = np.empty((2, S, D), np.float32)
    for c in cores:
        out[c // 4, (c % 4) * TOK:(c % 4 + 1) * TOK, :] = xT[c].T
    return out
```
